# Optimizing a Trainium2 kernel written in Bass

```python
import math
import jax, jax.numpy as jnp
from jax import lax
import numpy as np

D_MODEL = 4096
BATCH = 4
SEQ = 4096
DEPTH = 2

CTX_LEN = 256
GRID_W = 64
D_S5 = D_MODEL // 4
S5_GROUP_DIM = 16
S5_GROUPS = D_S5 // S5_GROUP_DIM
S5_STATE = 64
D_FT = D_MODEL // 4
FT_GROUPS = 4
FT_GROUP_DIM = D_FT // FT_GROUPS
D_IN = D_S5 + D_FT + 2 * D_MODEL
N_EXPERTS = 16
CAPACITY_FACTOR = 2
D_EXPERT = D_MODEL // 4
N_MOD = 6
RMS_EPS = 1e-6
POS_BASE = 10000.0
DT_MIN = 1e-3
DT_MAX = 1e-1
LAMBDA_RE_MAX = -1e-4

kernel_name = "hybrid_s5_fnet_ec_moe_dit"


def rmsnorm(x, g):
    xf = x.astype(jnp.float32)
    y = xf * lax.rsqrt(jnp.mean(xf * xf, axis=-1, keepdims=True) + RMS_EPS)
    return (y * g.astype(jnp.float32)).astype(x.dtype)


def modulate(h, shift, scale):
    return h * (1 + scale) + shift


def grid_posembed(n_tok, d):
    rows = n_tok // GRID_W
    row = jnp.broadcast_to(jnp.arange(rows, dtype=jnp.float32)[:, None], (rows, GRID_W)).reshape(-1)
    col = jnp.broadcast_to(jnp.arange(GRID_W, dtype=jnp.float32)[None, :], (rows, GRID_W)).reshape(-1)
    quarter = d // 4
    inv_freq = POS_BASE ** (-jnp.arange(quarter, dtype=jnp.float32) / quarter)
    ang_r = row[:, None] * inv_freq
    ang_c = col[:, None] * inv_freq
    return jnp.concatenate([jnp.sin(ang_r), jnp.cos(ang_r), jnp.sin(ang_c), jnp.cos(ang_c)], axis=-1)


def s5_discretise(lam_re, lam_im, log_dt, b_re, b_im):
    lr = jnp.minimum(lam_re.astype(jnp.float32), LAMBDA_RE_MAX)
    li = lam_im.astype(jnp.float32)
    dt = jnp.exp(log_dt.astype(jnp.float32))[:, None]
    mag = jnp.exp(lr * dt)
    a_re = mag * jnp.cos(li * dt)
    a_im = mag * jnp.sin(li * dt)
    num_re, num_im = a_re - 1.0, a_im
    den = lr * lr + li * li
    f_re = (num_re * lr + num_im * li) / den
    f_im = (num_im * lr - num_re * li) / den
    br, bi = b_re.astype(jnp.float32), b_im.astype(jnp.float32)
    bb_re = f_re[..., None] * br - f_im[..., None] * bi
    bb_im = f_re[..., None] * bi + f_im[..., None] * br
    return a_re, a_im, bb_re, bb_im


def _linear_recurrence_combine(e1, e2):
    a1r, a1i, b1r, b1i = e1
    a2r, a2i, b2r, b2i = e2
    return (a1r * a2r - a1i * a2i,
            a1r * a2i + a1i * a2r,
            a2r * b1r - a2i * b1i + b2r,
            a2r * b1i + a2i * b1r + b2i)


def diag_scan(ug, a_re, a_im, bb_re, bb_im, h0):
    bu_re = jnp.einsum('gpn,blgn->blgp', bb_re, ug)
    bu_im = jnp.einsum('gpn,blgn->blgp', bb_im, ug)
    if h0 is not None:
        h_re, h_im = h0
        bu_re = bu_re.at[:, 0].add(a_re * h_re - a_im * h_im)
        bu_im = bu_im.at[:, 0].add(a_re * h_im + a_im * h_re)
    l = ug.shape[1]
    ar = jnp.broadcast_to(a_re[None, None], (1, l) + a_re.shape)
    ai = jnp.broadcast_to(a_im[None, None], (1, l) + a_im.shape)
    _, _, h_re, h_im = lax.associative_scan(_linear_recurrence_combine, (ar, ai, bu_re, bu_im), axis=1)
    return h_re, h_im


def s5_bidirectional(u, disc_f, disc_b, h0_f, h0_b):
    b, l, _ = u.shape
    ug = u.astype(jnp.float32).reshape(b, l, S5_GROUPS, S5_GROUP_DIM)
    st_f = diag_scan(ug, *disc_f, h0_f)
    st_b = diag_scan(ug[:, ::-1], *disc_b, h0_b)
    return ug, st_f, st_b


def s5_readout(h_re, h_im, c_re, c_im):
    return (jnp.einsum('gnp,blgp->blgn', c_re.astype(jnp.float32), h_re)
            - jnp.einsum('gnp,blgp->blgn', c_im.astype(jnp.float32), h_im))


def fourier_mix(v):
    b, l, _ = v.shape
    vg = v.astype(jnp.float32).reshape(b, l, FT_GROUPS, FT_GROUP_DIM)
    y = jnp.fft.fft2(vg, axes=(1, 3), norm='ortho').real
    return y.reshape(b, l, D_FT).astype(v.dtype)


def hybrid_mixer(p, ug, st_f, st_b, c_f, c_b, s5_d, w_glu, w_s5_out, w_ft_out, w_out):
    b, l, _ = p.shape
    dtype = p.dtype
    d_skip = s5_d.astype(jnp.float32).reshape(S5_GROUPS, S5_GROUP_DIM)
    y = s5_readout(*st_f, *c_f) + s5_readout(*st_b, *c_b)[:, ::-1] + d_skip * ug
    y = jax.nn.gelu(y.reshape(b, l, D_S5).astype(dtype))
    y_s5 = (y * jax.nn.sigmoid(y @ w_glu)) @ w_s5_out
    y_ft = fourier_mix(p[..., D_S5:D_S5 + D_FT]) @ w_ft_out
    g_s5 = jax.nn.sigmoid(p[..., D_S5 + D_FT:D_S5 + D_FT + D_MODEL])
    g_ft = jax.nn.sigmoid(p[..., D_S5 + D_FT + D_MODEL:])
    return (g_s5 * y_s5 + g_ft * y_ft) @ w_out


def expert_choice_ffn(h, w_router, w_gate, w_up, w_down):
    b, n, _ = h.shape
    cap = CAPACITY_FACTOR * n // N_EXPERTS
    aff = jax.nn.softmax((h @ w_router).astype(jnp.float32), axis=-1)
    gates, idx = lax.top_k(jnp.swapaxes(aff, 1, 2), cap)
    bidx = jnp.arange(b)[:, None, None]
    xg = h[bidx, idx]
    hid = jax.nn.silu(jnp.einsum('becd,edf->becf', xg, w_gate)) * jnp.einsum('becd,edf->becf', xg, w_up)
    ye = jnp.einsum('becf,efd->becd', hid, w_down) * gates[..., None].astype(h.dtype)
    return jnp.zeros_like(h).at[bidx, idx].add(ye)


def setup_inputs(seed: int = 0) -> dict:
    key = jax.random.key(seed)
    k = jax.random.split(key, 26)
    d, g, p, n, e, f = D_MODEL, S5_GROUPS, S5_STATE, S5_GROUP_DIM, N_EXPERTS, D_EXPERT

    def nrm(i, shape, std):
        return std * jax.random.normal(k[i], shape, jnp.float32)

    state_idx = jnp.arange(p, dtype=jnp.float32)
    return {
        'x': nrm(0, (BATCH, SEQ, d), 1.0),
        'c': nrm(1, (BATCH, d), 1.0),
        'ctx': nrm(2, (BATCH, CTX_LEN, d), 1.0),
        'c_ctx': nrm(3, (d,), 1.0),
        'ada_w': nrm(4, (DEPTH, d, N_MOD * d), 0.5 * d ** -0.5),
        'ada_b': nrm(5, (DEPTH, N_MOD * d), 0.01),
        'norm_mix_g': 1.0 + nrm(6, (DEPTH, d), 0.02),
        'norm_ffn_g': 1.0 + nrm(7, (DEPTH, d), 0.02),
        'w_in': nrm(8, (DEPTH, d, D_IN), d ** -0.5),
        's5_lam_re': -0.5 + nrm(9, (DEPTH, 2, g, p), 0.01),
        's5_lam_im': math.pi * state_idx + nrm(10, (DEPTH, 2, g, p), 0.01),
        's5_log_dt': jax.random.uniform(k[11], (DEPTH, 2, g), jnp.float32, math.log(DT_MIN), math.log(DT_MAX)),
        's5_b_re': nrm(12, (DEPTH, 2, g, p, n), (2 * n) ** -0.5),
        's5_b_im': nrm(13, (DEPTH, 2, g, p, n), (2 * n) ** -0.5),
        's5_c_re': nrm(14, (DEPTH, 2, g, n, p), (2 * p) ** -0.5),
        's5_c_im': nrm(15, (DEPTH, 2, g, n, p), (2 * p) ** -0.5),
        's5_d': nrm(16, (DEPTH, D_S5), 1.0),
        'w_glu': nrm(17, (DEPTH, D_S5, D_S5), D_S5 ** -0.5),
        'w_s5_out': nrm(18, (DEPTH, D_S5, d), D_S5 ** -0.5),
        'w_ft_out': nrm(19, (DEPTH, D_FT, d), D_FT ** -0.5),
        'w_out': nrm(20, (DEPTH, d, d), d ** -0.5),
        'w_router': nrm(21, (DEPTH, d, e), d ** -0.5),
        'w_gate': nrm(22, (DEPTH, e, d, f), d ** -0.5),
        'w_up': nrm(23, (DEPTH, e, d, f), d ** -0.5),
        'w_down': nrm(24, (DEPTH, e, f, d), f ** -0.5),
        'norm_final_g': 1.0 + nrm(25, (d,), 0.02),
    }


def reference(x, c, ctx, c_ctx, ada_w, ada_b, norm_mix_g, norm_ffn_g, w_in,
              s5_lam_re, s5_lam_im, s5_log_dt, s5_b_re, s5_b_im, s5_c_re, s5_c_im, s5_d,
              w_glu, w_s5_out, w_ft_out, w_out, w_router, w_gate, w_up, w_down, norm_final_g):
    x = x + grid_posembed(x.shape[1], x.shape[2]).astype(x.dtype)[None]
    silu_c = jax.nn.silu(c)
    silu_cc = jax.nn.silu(c_ctx)
    for i in range(DEPTH):
        last = i == DEPTH - 1
        mod_x = (silu_c @ ada_w[i] + ada_b[i])[:, None, :]
        mod_c = (silu_cc @ ada_w[i] + ada_b[i])[None, None, :]
        sh_mx, sc_mx, g_mx, sh_fx, sc_fx, g_fx = jnp.split(mod_x, N_MOD, axis=-1)
        sh_mc, sc_mc, g_mc, sh_fc, sc_fc, g_fc = jnp.split(mod_c, N_MOD, axis=-1)
        disc_f = s5_discretise(s5_lam_re[i, 0], s5_lam_im[i, 0], s5_log_dt[i, 0], s5_b_re[i, 0], s5_b_im[i, 0])
        disc_b = s5_discretise(s5_lam_re[i, 1], s5_lam_im[i, 1], s5_log_dt[i, 1], s5_b_re[i, 1], s5_b_im[i, 1])
        c_f = (s5_c_re[i, 0], s5_c_im[i, 0])
        c_b = (s5_c_re[i, 1], s5_c_im[i, 1])
        mixer_w = (s5_d[i], w_glu[i], w_s5_out[i], w_ft_out[i], w_out[i])

        hc = modulate(rmsnorm(ctx, norm_mix_g[i]), sh_mc, sc_mc)
        pc = hc @ w_in[i][:, :D_S5] if last else hc @ w_in[i]
        ug_c, st_fc, st_bc = s5_bidirectional(pc[..., :D_S5], disc_f, disc_b, None, None)
        h0_f = (st_fc[0][:, -1], st_fc[1][:, -1])
        h0_b = (st_bc[0][:, -1], st_bc[1][:, -1])

        hx = modulate(rmsnorm(x, norm_mix_g[i]), sh_mx, sc_mx)
        px = hx @ w_in[i]
        ug_x, st_fx, st_bx = s5_bidirectional(px[..., :D_S5], disc_f, disc_b, h0_f, h0_b)
        x = x + g_mx * hybrid_mixer(px, ug_x, st_fx, st_bx, c_f, c_b, *mixer_w)
        hx2 = modulate(rmsnorm(x, norm_ffn_g[i]), sh_fx, sc_fx)
        x = x + g_fx * expert_choice_ffn(hx2, w_router[i], w_gate[i], w_up[i], w_down[i])

        if not last:
            ctx = ctx + g_mc * hybrid_mixer(pc, ug_c, st_fc, st_bc, c_f, c_b, *mixer_w)
            hc2 = modulate(rmsnorm(ctx, norm_ffn_g[i]), sh_fc, sc_fc)
            ctx = ctx + g_fc * expert_choice_ffn(hc2, w_router[i], w_gate[i], w_up[i], w_down[i])
    return rmsnorm(x, norm_final_g)
```

```python
import math
from contextlib import ExitStack
import numpy as np
import ml_dtypes
import concourse.bass as bass
import concourse.mybir as mybir
from concourse.ap import AP
from concourse.bass_utils import run_bass_kernel_spmd

F32 = mybir.dt.float32
BF16 = mybir.dt.bfloat16
I32 = mybir.dt.int32
U32 = mybir.dt.uint32
AF = mybir.ActivationFunctionType
ALU = mybir.AluOpType
NCORES = 8
TWO_PI = 2.0 * math.pi


class Cfg:
    def __init__(s, D=4096, L=4096, LC=256, E=16, GRID_W=64, DEPTH=2):
        s.D, s.L, s.LC, s.E, s.GRID_W, s.DEPTH = D, L, LC, E, GRID_W, DEPTH
        s.DS = D // 4
        s.DF = D // 4
        s.G = s.DS // 16
        s.P = 64
        s.N = 16
        s.GD = s.DF // 4
        s.DIN = s.DS + s.DF + 2 * D
        s.F = D // 4
        s.KC = D // 128
        s.SC = s.DS // 128
        s.NGP = s.G // 2
        s.FC = s.F // 128
        s.MOE_CW = 2048


class Sem:
    def __init__(s, nc, name):
        s.h = nc.alloc_semaphore(name)
        s.v = 0


_KCTX = []


def _has_update(ins):
    h = ins.ins.has_update
    return h() if callable(h) else bool(h)


def sig(ins, sem, n=1):
    if _has_update(ins):
        k = _KCTX[-1]
        et = str(ins.ins.engine)
        if "DVE" in et:
            ins = k.dve.memset(k.scr[:, 0:1], 0.0)
        elif "Pool" in et or "POOL" in et:
            ins = k.pool.memset(k.scr[:, 1:2], 0.0)
        elif "Act" in et or "ACT" in et:
            ins = k.act.activation(out=k.scr[:, 2:3], in_=k.scr[:, 3:4], func=AF.Copy)
        elif "PE" in et or "Tensor" in et:
            ins = k.pe.matmul(k.ps[7][0:8, 0:8], lhsT=k.scrb[:, 0:8], rhs=k.scrb[:, 0:8], start=True, stop=True)
        else:
            raise RuntimeError(f"cannot add 2nd sem update on engine {et}")
    ins.then_inc(sem.h, n)
    sem.v += n
    return sem.v


class Ring:
    def __init__(self, k, name, n):
        self.n = n
        self.full = [k.sem(f"{name}f{i}") for i in range(n)]
        self.free = [k.sem(f"{name}e{i}") for i in range(n)]
        self.wi = 0
        self.ri = 0
        self.need = [0] * n
        self.ready = []
        self.cur_w = 0
        self.cur_r = 0

    def wslot(self, *engs):
        s = self.wi % self.n
        self.wi += 1
        if self.need[s] > 0:
            for e in engs:
                e.wait_ge(self.free[s].h, self.need[s])
        self.cur_w = s
        return s

    def wdone(self, ins, inc=1):
        sig(ins, self.full[self.cur_w], inc)

    def wcommit(self):
        self.ready.append((self.cur_w, self.full[self.cur_w].v))

    def rslot(self, *engs):
        s, v = self.ready[self.ri]
        self.ri += 1
        for e in engs:
            e.wait_ge(self.full[s].h, v)
        self.cur_r = s
        return s

    def rdone(self, ins):
        s = self.cur_r
        sig(ins, self.free[s], 1)
        self.need[s] = self.free[s].v


class K:
    def __init__(self, nc, cfg):
        self.nc = nc
        self.cfg = cfg
        self.pe, self.act, self.dve, self.pool, self.sp = nc.tensor, nc.scalar, nc.vector, nc.gpsimd, nc.sync
        self._sems = []
        self._free = []
        self.nsem = 0
        self.ps = [nc.alloc_psum_tensor(f"psb{i}", [128, 512], F32) for i in range(8)]
        self.bar = Sem(nc, "bar")
        self.scr = nc.alloc_sbuf_tensor("barscr", [128, 8], F32)
        self.scrb = nc.alloc_sbuf_tensor("barscrb", [128, 8], BF16)
        _KCTX.append(self)
        self._fs = {}
        self.pool_busy = False
        self.wsem = Sem(nc, "wready")
        self.csem = Sem(nc, "wcast")
        self.wready = {}
        i0 = Sem(nc, "init0")
        self.dve.memset(self.scr[:, :], 0.0)
        self.dve.memset(self.scrb[:, :], 0.0).then_inc(i0.h, 1)
        for e in (self.pe, self.act, self.pool):
            e.wait_ge(i0.h, 1)

    def fence(self, eng, ins):
        key = str(eng.engine)
        if key not in self._fs:
            self._fs[key] = Sem(self.nc, "fence" + key[-3:])
        f = self._fs[key]
        sig(ins, f)
        eng.wait_ge(f.h, f.v)
        return ins

    def sem(self, name="s"):
        if self._free:
            return self._free.pop()
        self.nsem += 1
        s = Sem(self.nc, f"{name}_{self.nsem}")
        self._sems.append(s)
        return s

    def release_all(self):
        self._free = list(self._sems)

    def barrier(self):
        b = self.bar
        sig(self.dve.memset(self.scr[:, 0:1], 0.0), b)
        if not self.pool_busy:
            sig(self.pool.memset(self.scr[:, 1:2], 0.0), b)
        sig(self.act.activation(out=self.scr[:, 2:3], in_=self.scr[:, 3:4], func=AF.Copy), b)
        sig(self.pe.matmul(self.ps[7][0:8, 0:8], lhsT=self.scrb[:, 0:8], rhs=self.scrb[:, 0:8], start=True, stop=True), b)
        for e in (self.pe, self.act, self.dve, self.pool, self.sp):
            if e is self.pool and self.pool_busy:
                continue
            e.wait_ge(b.h, b.v)
        self.release_all()


def dview(t, pat, **kw):
    return t.ap().rearrange(pat, **kw)


def cast_allgather(k, name, shard, rows, cols, dt_in=F32):
    nc = k.nc
    rs = rows // NCORES
    odt = BF16 if dt_in == F32 else dt_in
    xb = nc.dram_tensor(name + "_sb", [rs, cols], odt)
    full = nc.dram_tensor(name + "_full", [rows, cols], odt)
    gp = k.pool
    ds = k.csem
    per_row = (cols + 2047) // 2048
    rb = max(1, min(rs, 6144 // per_row))
    for r0 in range(0, rs, rb):
        r1 = min(rs, r0 + rb)
        if dt_in == F32:
            sig(gp.dma_start(out=xb[r0:r1, :], in_=shard[r0:r1, :], max_dma_last_dim=8192), ds, 16)
        else:
            sig(gp.dma_start(out=xb[r0:r1, :], in_=shard[r0:r1, :]), ds, 16)
    gp.wait_ge(ds.h, ds.v)
    cs = k.wsem
    sig(gp.collective_compute("AllGather", ALU.bypass, replica_groups=[list(range(NCORES))],
                              ins=[xb.ap().opt()], outs=[full.ap().opt()]), cs, 1)
    gp.wait_ge(cs.h, cs.v)
    k.wready[name] = cs.v
    return full


def copy_allgather_f32(k, name, shard, rows, cols):
    nc = k.nc
    rs = rows // NCORES
    xb = nc.dram_tensor(name + "_sb", [rs, cols], F32)
    full = nc.dram_tensor(name + "_full", [rows, cols], F32)
    gp = k.pool
    ds = k.sem("cp")
    sig(gp.dma_start(out=xb[:, :], in_=shard[:, :]), ds, 16)
    gp.wait_ge(ds.h, ds.v)
    cs = k.sem("cc")
    sig(gp.collective_compute("AllGather", ALU.bypass, replica_groups=[list(range(NCORES))],
                              ins=[xb.ap().opt()], outs=[full.ap().opt()]), cs, 1)
    gp.wait_ge(cs.h, cs.v)
    return full


def phase_mod(k, es, li, scT, ada_w, ada_b_in, modt):
    nc, c = k.nc, k.cfg
    KC = c.KC
    NJ = 6 * KC
    CB = 512
    JB = CB // 128
    wr = Ring(k, "adaw", 2)
    wt = [es.enter_context(nc.sbuf_tensor(f"adaw{li}_{i}", [128, KC, CB], BF16)) for i in range(2)]
    bt = es.enter_context(nc.sbuf_tensor(f"adab{li}", [128, NJ], F32))
    bs = k.sem("adab")
    with nc.allow_non_contiguous_dma(reason="small bias transpose"):
        sig(k.sp.dma_start(out=bt[:, :], in_=ada_b_in.ap()[li].rearrange("(j p) -> p j", p=128)), bs, 16)
    wv = dview(ada_w, "(kc p) n -> p kc n", p=128)
    ps = k.ps[0]
    pes = k.sem("modpe")
    for jb in range(NJ // JB):
        s = wr.wslot(k.sp)
        wr.wdone(k.sp.dma_start(out=wt[s][:, :, :], in_=wv[:, :, jb * CB:(jb + 1) * CB]), 16)
        wr.wcommit()
        s = wr.rslot(k.pe)
        last = None
        for jj in range(JB):
            jo = jb * JB + jj
            for kc in range(KC):
                last = k.pe.matmul(ps[:, 2 * jo:2 * jo + 2], lhsT=wt[s][:, kc, jj * 128:(jj + 1) * 128],
                                   rhs=scT[:, kc, :], start=(kc == 0), stop=(kc == KC - 1))
        wr.rdone(last)
    sig(last, pes)
    k.dve.wait_ge(pes.h, pes.v)
    k.dve.wait_ge(bs.h, bs.v)
    pv = ps[:, 0:2 * NJ].rearrange("p (j r) -> p r j", r=2)
    for r in range(2):
        k.fence(k.dve, k.dve.tensor_tensor(out=modt[:, r, :], in0=pv[:, r, :], in1=bt[:, :], op=ALU.add))


class NormRes:
    def __init__(self, k, es, tag, TB, out_dt=BF16):
        nc, c = k.nc, k.cfg
        self.TB = TB
        self.xblk = es.enter_context(nc.sbuf_tensor(f"{tag}_xb", [128, c.KC, TB], F32))
        self.hT = es.enter_context(nc.sbuf_tensor(f"{tag}_hT", [128, c.KC, TB], out_dt))
        self.sq = [es.enter_context(nc.sbuf_tensor(f"{tag}_sq{i}", [128, TB], F32)) for i in range(2)]
        self.tmp = [es.enter_context(nc.sbuf_tensor(f"{tag}_tm{i}", [128, TB], F32)) for i in range(2)]
        self.rstd = es.enter_context(nc.sbuf_tensor(f"{tag}_rs", [128, TB], F32))
        self.sqr = Ring(k, tag + "sq", 2)
        self.tmr = Ring(k, tag + "tm", 2)
        self.x_full = k.sem(tag + "xf")
        self.x_free = k.sem(tag + "xe")
        self.x_need = 0
        self.h_full = k.sem(tag + "hf")
        self.h_free = k.sem(tag + "he")
        self.h_need = 0
        self.ss_full = k.sem(tag + "ssf")
        self.rs_full = k.sem(tag + "rsf")
        self.ss_free = k.sem(tag + "sse")
        self.ss_need = 0


def emit_norm(k, nr, xsrc, t0, Acol, Bcol, ones32, eps_col, pss):
    c = k.cfg
    KC, TB = c.KC, nr.TB
    xv = dview(xsrc, "(kc p) l -> p kc l", p=128)
    if nr.x_need:
        k.sp.wait_ge(nr.x_free.h, nr.x_need)
    sig(k.sp.dma_start(out=nr.xblk[:, :, :], in_=xv[:, :, t0:t0 + TB]), nr.x_full, 16)
    xf = nr.x_full.v
    k.act.wait_ge(nr.x_full.h, xf)
    k.dve.wait_ge(nr.x_full.h, xf)
    if nr.ss_need:
        k.pe.wait_ge(nr.ss_free.h, nr.ss_need)
    last = None
    for kc in range(KC):
        s = nr.sqr.wslot(k.act)
        nr.sqr.wdone(k.act.activation(out=nr.sq[s][:, :], in_=nr.xblk[:, kc, :], func=AF.Square))
        nr.sqr.wcommit()
        s = nr.sqr.rslot(k.pe)
        last = k.pe.matmul(pss[:, 0:TB], lhsT=ones32[:, :], rhs=nr.sq[s][:, :], start=(kc == 0), stop=(kc == KC - 1))
        nr.sqr.rdone(last)
    sig(last, nr.ss_full)
    k.act.wait_ge(nr.ss_full.h, nr.ss_full.v)
    a = k.act.activation(out=nr.rstd[:, :], in_=pss[:, 0:TB], func=AF.Sqrt, bias=eps_col[:, 0:1], scale=1.0 / c.D)
    sig(a, nr.ss_free)
    nr.ss_need = nr.ss_free.v
    sig(a, nr.rs_full)
    k.dve.wait_ge(nr.rs_full.h, nr.rs_full.v)
    k.dve.reciprocal(out=nr.rstd[:, :], in_=nr.rstd[:, :])
    if nr.h_need:
        k.act.wait_ge(nr.h_free.h, nr.h_need)
    lastd = None
    for kc in range(KC):
        s = nr.tmr.wslot(k.dve)
        lastd = k.dve.scalar_tensor_tensor(out=nr.tmp[s][:, :], in0=nr.xblk[:, kc, :], scalar=Acol[:, kc:kc + 1],
                                           in1=nr.rstd[:, :], op0=ALU.mult, op1=ALU.mult)
        nr.tmr.wdone(lastd)
        nr.tmr.wcommit()
        s = nr.tmr.rslot(k.act)
        a = k.act.activation(out=nr.hT[:, kc, :], in_=nr.tmp[s][:, :], func=AF.Identity, bias=Bcol[:, kc:kc + 1], scale=1.0)
        nr.tmr.rdone(a)
    sig(a, nr.h_full)
    sig(lastd, nr.x_free)
    sig(a, nr.x_free)
    nr.x_need = nr.x_free.v
    return nr.h_full.v


def norm_consumed(k, nr, ins):
    sig(ins, nr.h_free)
    nr.h_need = nr.h_free.v


class LinW:
    def __init__(self, k, es, tag, KCin, GW, nbuf=2):
        nc = k.nc
        self.KCin, self.GW = KCin, GW
        self.ring = Ring(k, tag, nbuf)
        self.t = [es.enter_context(nc.sbuf_tensor(f"{tag}_w{i}", [128, KCin, GW], BF16)) for i in range(nbuf)]

    def load(self, k, w, row0, col0, eng=None):
        eng = eng or k.sp
        s = self.ring.wslot(eng)
        wv = w.ap()[row0:row0 + self.KCin * 128, col0:col0 + self.GW].rearrange("(kc p) n -> p kc n", p=128)
        self.ring.wdone(eng.dma_start(out=self.t[s][:, :, :], in_=wv), 16)
        self.ring.wcommit()

    def get(self, k):
        s = self.ring.rslot(k.pe)
        return self.t[s]

    def done(self, ins):
        self.ring.rdone(ins)


class PsRing:
    def __init__(self, k, tag, banks):
        self.banks = banks
        self.ring = Ring(k, tag, len(banks))

    def wslot(self, k):
        s = self.ring.wslot(k.pe)
        return k.ps[self.banks[s]]

    def wdone(self, ins):
        self.ring.wdone(ins)
        self.ring.wcommit()

    def rslot(self, k, *engs):
        s = self.ring.rslot(*engs)
        return k.ps[self.banks[s]]

    def rdone(self, ins):
        self.ring.rdone(ins)


class OutStage:
    def __init__(self, k, es, tag, shape, dt, nbuf=3):
        nc = k.nc
        self.ring = Ring(k, tag, nbuf)
        self.t = [es.enter_context(nc.sbuf_tensor(f"{tag}_o{i}", shape, dt)) for i in range(nbuf)]

    def wslot(self, *engs):
        s = self.ring.wslot(*engs)
        return self.t[s]

    def store(self, k, ins, dst_ap, src_fn=None, eng=None):
        eng = eng or k.sp
        self.ring.wdone(ins)
        self.ring.wcommit()
        s = self.ring.rslot(eng)
        src = self.t[s][:, :] if src_fn is None else src_fn(self.t[s])
        d = eng.dma_start(out=dst_ap, in_=src)
        sig(d, self.ring.free[s], 16)
        self.ring.need[s] = self.ring.free[s].v
        return d

    def drain(self, k, eng=None):
        eng = eng or k.sp
        for s in range(self.ring.n):
            if self.ring.need[s]:
                eng.wait_ge(self.ring.free[s].h, self.ring.need[s])


def phase_in(k, tag, xsrc, L, Acol, Bcol, w_in, consts, outs, ncols_chunks):
    nc, c = k.nc, k.cfg
    TB = min(512, L)
    with ExitStack() as es:
        nr = NormRes(k, es, tag + "n", TB)
        GW = 512
        lw = LinW(k, es, tag + "w", c.KC, GW, 2)
        pr = PsRing(k, tag + "p", [1, 2, 3, 4])
        st = OutStage(k, es, tag + "o", [128, TB], BF16, 4)
        uT, vT, g1T, g2T = outs
        SCn, FCn = c.DS // 128, c.DF // 128
        ev = 0
        for tb in range(L // TB):
            t0 = tb * TB
            hv = emit_norm(k, nr, xsrc, t0, Acol, Bcol, consts["ones32"], consts["eps"], k.ps[0])
            k.pe.wait_ge(nr.h_full.h, hv)
            last = None
            for jg in range(0, ncols_chunks, GW // 128):
                njj = min(GW // 128, ncols_chunks - jg)
                lw.load(k, w_in, 0, jg * 128)
                wt = lw.get(k)
                for jj in range(njj):
                    jo = jg + jj
                    ps = pr.wslot(k)
                    for kc in range(c.KC):
                        last = k.pe.matmul(ps[:, 0:TB], lhsT=wt[:, kc, jj * 128:(jj + 1) * 128], rhs=nr.hT[:, kc, :],
                                           start=(kc == 0), stop=(kc == c.KC - 1))
                    pr.wdone(last)
                    if jo < SCn:
                        dst = uT.ap()[jo * 128:(jo + 1) * 128, t0:t0 + TB]
                        func = None
                    elif jo < SCn + FCn:
                        dst = vT.ap()[(jo - SCn) * 128:(jo - SCn + 1) * 128, t0:t0 + TB]
                        func = None
                    elif jo < SCn + FCn + c.KC:
                        j2 = jo - SCn - FCn
                        dst = g1T.ap()[j2 * 128:(j2 + 1) * 128, t0:t0 + TB]
                        func = AF.Sigmoid
                    else:
                        j2 = jo - SCn - FCn - c.KC
                        dst = g2T.ap()[j2 * 128:(j2 + 1) * 128, t0:t0 + TB]
                        func = AF.Sigmoid
                    if func is None and (ev % 2 == 0):
                        eng = k.dve
                        ps = pr.rslot(k, eng)
                        o = st.wslot(eng)
                        ins = eng.tensor_copy(out=o[:, :], in_=ps[:, 0:TB])
                    else:
                        eng = k.act
                        ps = pr.rslot(k, eng)
                        o = st.wslot(eng)
                        ins = eng.activation(out=o[:, :], in_=ps[:, 0:TB], func=(func or AF.Copy))
                    ev += 1
                    pr.rdone(ins)
                    st.store(k, ins, dst)
                lw.done(last)
            norm_consumed(k, nr, last)
        st.drain(k)
    k.barrier()


BIG_W = [
    ("ada_w", lambda c: c.D, lambda c: 6 * c.D),
    ("w_in", lambda c: c.D, lambda c: c.DIN),
    ("w_glu", lambda c: c.DS, lambda c: c.DS),
    ("w_s5_out", lambda c: c.DS, lambda c: c.D),
    ("w_ft_out", lambda c: c.DF, lambda c: c.D),
    ("w_out", lambda c: c.D, lambda c: c.D),
    ("w_gate", lambda c: c.E * c.D, lambda c: c.F),
    ("w_up", lambda c: c.E * c.D, lambda c: c.F),
    ("w_down", lambda c: c.E * c.F, lambda c: c.D),
]


def build_program(cfg, stop_after=None, debug_outs=(), use_pe=True):
    c = cfg
    nc = bass.Bass("TRN2", target_bir_lowering=False)
    k = K(nc, c)
    D, L, LC, KC = c.D, c.L, c.LC, c.KC
    dbg = {}

    def dram(name, shape, dt):
        if name in debug_outs:
            t = nc.dram_tensor(name, shape, dt, kind="ExternalOutput")
            dbg[name] = t
            return t
        return nc.dram_tensor(name, shape, dt)

    xT_in = nc.dram_tensor("xT", [D, L], F32, kind="ExternalInput")
    cxT_in = nc.dram_tensor("cxT", [D, LC], F32, kind="ExternalInput")
    cvec = nc.dram_tensor("cvec", [2, D], F32, kind="ExternalInput")
    wsh = {}
    for li in range(c.DEPTH):
        for name, rf, cf in BIG_W:
            wsh[(name, li)] = nc.dram_tensor(f"{name}{li}", [rf(c) // NCORES, cf(c)], F32, kind="ExternalInput")
    ada_b = nc.dram_tensor("ada_b", [c.DEPTH, 6 * D], F32, kind="ExternalInput")
    norm_mix_g = nc.dram_tensor("norm_mix_g", [c.DEPTH, D], F32, kind="ExternalInput")
    norm_ffn_g = nc.dram_tensor("norm_ffn_g", [c.DEPTH, D], F32, kind="ExternalInput")
    norm_final_g = nc.dram_tensor("norm_final_g", [1, D], F32, kind="ExternalInput")
    ident32_in = nc.dram_tensor("ident32", [128, 128], F32, kind="ExternalInput")
    iota_in = nc.dram_tensor("iota512", [128, 512], F32, kind="ExternalInput")
    prm = {}
    for name, shp in (("lam_re", [c.DEPTH, 2, c.G, c.P]), ("lam_im", [c.DEPTH, 2, c.G, c.P]), ("log_dt", [c.DEPTH, 2, c.G]),
                      ("b_re", [c.DEPTH, 2, c.G, c.P, c.N]), ("b_im", [c.DEPTH, 2, c.G, c.P, c.N]),
                      ("c_re", [c.DEPTH, 2, c.G, c.N, c.P]), ("c_im", [c.DEPTH, 2, c.G, c.N, c.P])):
        prm[name] = nc.dram_tensor("s5_" + name, shp, F32, kind="ExternalInput")
    s5_d_in = nc.dram_tensor("s5_d", [c.DEPTH, c.DS], F32, kind="ExternalInput")
    CD_in = nc.dram_tensor("CD", [c.DF, 2 * c.DF], BF16, kind="ExternalInput")
    w_router_in = nc.dram_tensor("w_router", [c.DEPTH, D, c.E], F32, kind="ExternalInput")
    rowcol_in = nc.dram_tensor("rowcol", [128, 2, L], F32, kind="ExternalInput")
    invf_in = nc.dram_tensor("invf", [1, D], F32, kind="ExternalInput")
    DLc_in = nc.dram_tensor("DLc", [LC, 2 * LC], BF16, kind="ExternalInput")
    DLx_sh = nc.dram_tensor("DLx", [L // NCORES, 2 * L], BF16, kind="ExternalInput")
    outT = nc.dram_tensor("outT", [D, L], F32, kind="ExternalOutput")

    ones32 = nc.alloc_sbuf_tensor("ones32", [128, 128], F32)
    eps_col = nc.alloc_sbuf_tensor("eps_col", [128, 1], F32)
    ident32 = nc.alloc_sbuf_tensor("ident32s", [128, 128], F32)
    ident16 = nc.alloc_sbuf_tensor("ident16s", [128, 128], BF16)
    iota = nc.alloc_sbuf_tensor("iota_s", [128, 512], F32)
    pi_col = nc.alloc_sbuf_tensor("pi_col", [128, 1], F32)
    hpi_col = nc.alloc_sbuf_tensor("hpi_col", [128, 1], F32)
    consts = {"ones32": ones32, "eps": eps_col, "ident32": ident32, "ident16": ident16, "iota": iota, "pi": pi_col, "hpi": hpi_col}
    cs0 = k.sem("c0")
    k.dve.memset(ones32[:, :], 1.0)
    k.dve.memset(eps_col[:, :], 1e-6)
    k.dve.memset(pi_col[:, :], math.pi)
    k.dve.memset(hpi_col[:, :], 0.5 * math.pi)
    sig(k.sp.dma_start(out=ident32[:, :], in_=ident32_in[:, :]), cs0, 16)
    sig(k.sp.dma_start(out=iota[:, :], in_=iota_in[:, :]), cs0, 16)
    k.dve.wait_ge(cs0.h, cs0.v)
    k.dve.tensor_copy(out=ident16[:, :], in_=ident32[:, :])

    W = {}
    for li in range(c.DEPTH):
        for name, rf, cf in BIG_W:
            W[(name, li)] = cast_allgather(k, f"{name}{li}", wsh[(name, li)], rf(c), cf(c))
    DLx = cast_allgather(k, "DLx", DLx_sh, L, 2 * L, dt_in=BF16)
    k.pool_busy = True

    def need_w(name, li_):
        k.sp.wait_ge(k.wsem.h, k.wready[f"{name}{li_}"])

    scT = nc.alloc_sbuf_tensor("scT", [128, KC, 2], BF16)
    ctmp = nc.alloc_sbuf_tensor("ctmp", [128, KC, 2], F32)
    s1 = k.sem("sc")
    with nc.allow_non_contiguous_dma(reason="tiny c transpose"):
        for r in range(2):
            sig(k.sp.dma_start(out=ctmp[:, :, r], in_=cvec.ap()[r].rearrange("(kc p) -> p kc", p=128)), s1, 16)
    k.act.wait_ge(s1.h, s1.v)
    k.act.activation(out=scT[:, :, :], in_=ctmp[:, :, :], func=AF.Silu)
    gm = nc.alloc_sbuf_tensor("gm", [128, c.DEPTH, KC], F32)
    gf = nc.alloc_sbuf_tensor("gf", [128, c.DEPTH, KC], F32)
    gfin = nc.alloc_sbuf_tensor("gfin", [128, 1, KC], F32)
    with nc.allow_non_contiguous_dma(reason="tiny gain transpose"):
        for l_ in range(c.DEPTH):
            sig(k.sp.dma_start(out=gm[:, l_, :], in_=norm_mix_g.ap()[l_].rearrange("(kc p) -> p kc", p=128)), s1, 16)
            sig(k.sp.dma_start(out=gf[:, l_, :], in_=norm_ffn_g.ap()[l_].rearrange("(kc p) -> p kc", p=128)), s1, 16)
        sig(k.sp.dma_start(out=gfin[:, 0, :], in_=norm_final_g.ap()[0].rearrange("(kc p) -> p kc", p=128)), s1, 16)
    k.dve.wait_ge(s1.h, s1.v)
    k.barrier()

    xres = dram("xres", [D, L], F32)
    cres = dram("cres", [D, LC], F32)
    s2 = k.sem("cpy")
    sig(k.sp.dma_start(out=cres[:, :], in_=cxT_in[:, :]), s2, 16)
    with ExitStack() as es0:
        rowcol = es0.enter_context(nc.sbuf_tensor("rowcol_s", [128, 2, L], F32))
        invf = es0.enter_context(nc.sbuf_tensor("invf_s", [128, KC], F32))
        sig(k.sp.dma_start(out=rowcol[:, :, :], in_=rowcol_in[:, :, :]), s2, 16)
        with nc.allow_non_contiguous_dma(reason="tiny"):
            sig(k.sp.dma_start(out=invf[:, :], in_=invf_in.ap()[0].rearrange("(kc p) -> p kc", p=128)), s2, 16)
        k.sp.wait_ge(s2.h, s2.v)
        k.dve.wait_ge(s2.h, s2.v)
        k.barrier()
        if use_pe:
            phase_posembed(k, "pe", xT_in, xres, L, rowcol, invf, consts)
        else:
            sig(k.sp.dma_start(out=xres[:, :], in_=xT_in[:, :]), s2, 16)
            k.sp.wait_ge(s2.h, s2.v)
            k.barrier()

    uT = dram("uT", [c.DS, L], BF16)
    vT = dram("vT", [c.DF, L], BF16)
    g1T = dram("g1T", [D, L], BF16)
    g2T = dram("g2T", [D, L], BF16)
    cuT = dram("cuT", [c.DS, LC], BF16)
    cvT = dram("cvT", [c.DF, LC], BF16)
    cg1T = dram("cg1T", [D, LC], BF16)
    cg2T = dram("cg2T", [D, LC], BF16)

    yT = dram("yT", [c.DS, L], BF16)
    yfT = dram("yfT", [c.DF, L], BF16)
    hx2 = dram("hx2", [L, D], BF16)
    chx2 = dram("chx2", [LC, D], BF16)
    NH_ = max(1, D // c.MOE_CW)
    xmoe = [dram(f"xmoe{h_}", [L, D // NH_], F32) for h_ in range(NH_)]
    cmoe = [dram(f"cmoe{h_}", [LC, D // NH_], F32) for h_ in range(NH_)]
    cyfT = dram("cyfT", [c.DF, LC], BF16)
    cyT = dram("cyT", [c.DS, LC], BF16)
    S5 = {"consts": consts,
          "sc": nc.alloc_sbuf_tensor("s5sc", [128, 2, 8, c.NGP], F32),
          "hcar": nc.alloc_sbuf_tensor("s5hcar", [128, 2, c.NGP, 2], F32)}
    dcols = nc.alloc_sbuf_tensor("dcols", [128, c.DEPTH, c.SC], F32)
    with nc.allow_non_contiguous_dma(reason="tiny"):
        for l_ in range(c.DEPTH):
            sig(k.sp.dma_start(out=dcols[:, l_, :], in_=s5_d_in.ap()[l_].rearrange("(kc p) -> p kc", p=128)), s2, 16)
    k.sp.wait_ge(s2.h, s2.v)
    modt = nc.alloc_sbuf_tensor("modt", [128, 2, 6 * KC], F32)
    AB = nc.alloc_sbuf_tensor("AB", [128, 2, 2, KC], F32)

    for li in range(c.DEPTH):
        last = li == c.DEPTH - 1
        with ExitStack() as es:
            if k.pool_busy:
                need_w("ada_w", li)
            phase_mod(k, es, li, scT, W[("ada_w", li)], ada_b, modt)
            for r in range(2):
                k.dve.scalar_tensor_tensor(out=AB[:, r, 0, :], in0=modt[:, r, 1 * KC:2 * KC], scalar=1.0, in1=gm[:, li, :],
                                           op0=ALU.add, op1=ALU.mult)
                k.dve.scalar_tensor_tensor(out=AB[:, r, 1, :], in0=modt[:, r, 4 * KC:5 * KC], scalar=1.0, in1=gf[:, li, :],
                                           op0=ALU.add, op1=ALU.mult)
        k.barrier()
        if stop_after == f"mod{li}":
            break
        if k.pool_busy:
            need_w("w_in", li)
        phase_in(k, f"ci{li}", cres, LC, AB[:, 1, 0, :], modt[:, 1, 0:KC], W[("w_in", li)], consts,
                 (cuT, cvT, cg1T, cg2T), (c.DS // 128) if last else (c.DIN // 128))
        phase_in(k, f"xi{li}", xres, L, AB[:, 0, 0, :], modt[:, 0, 0:KC], W[("w_in", li)], consts,
                 (uT, vT, g1T, g2T), c.DIN // 128)
        if stop_after == f"in{li}":
            break
        if k.pool_busy:
            k.pool_busy = False
            k.barrier()
        with ExitStack() as es5:
            S5["lB"] = [[es5.enter_context(nc.sbuf_tensor(f"lB{li}{d}{r}", [128, c.NGP, 128], BF16)) for r in range(2)] for d in range(2)]
            S5["lR"] = [[es5.enter_context(nc.sbuf_tensor(f"lR{li}{d}{r}", [128, c.NGP, 128], BF16)) for r in range(2)] for d in range(2)]
            s5_setup(k, li, prm, S5)
            k.dve.memset(S5["hcar"][:, :, :, :], 0.0)
            k.barrier()
            if "dbg_sc" in debug_outs and li == 0:
                ds_ = k.sem("dbg")
                t_ = nc.dram_tensor("dbg_sc", [128, 2 * 8 * c.NGP], F32, kind="ExternalOutput"); dbg["dbg_sc"] = t_
                sig(k.sp.dma_start(out=t_[:, :], in_=S5["sc"][:, :, :, :].rearrange("p a b c -> p (a b c)")), ds_, 16)
                for nm, tt in (("dbg_lB0", S5["lB"][0][0]), ("dbg_lB1", S5["lB"][0][1]), ("dbg_lR0", S5["lR"][0][0]), ("dbg_lR1", S5["lR"][1][1])):
                    t_ = nc.dram_tensor(nm, [128, c.NGP * 128], BF16, kind="ExternalOutput"); dbg[nm] = t_
                    sig(k.sp.dma_start(out=t_[:, :], in_=tt[:, :, :].rearrange("p a b -> p (a b)")), ds_, 16)
                k.sp.wait_ge(ds_.h, ds_.v)
                k.barrier()
            phase_s5(k, f"cs{li}", li, cuT, LC, S5, cyT, not last, dcols[:, li, :])
            phase_s5(k, f"xs{li}", li, uT, L, S5, yT, True, dcols[:, li, :])
        if stop_after == f"s5{li}":
            break
        if not last:
            phase_fft(k, f"cf{li}", cvT, LC, CD_in, DLc_in, cyfT)
        phase_fft(k, f"xf{li}", vT, L, CD_in, DLx, yfT)
        if stop_after == f"fft{li}":
            break
        if not last:
            phase_merge(k, f"cm{li}", LC, cyT, cyfT, cg1T, cg2T, cres, W, li, modt[:, 1, 2 * KC:3 * KC])
        phase_merge(k, f"xm{li}", L, yT, yfT, g1T, g2T, xres, W, li, modt[:, 0, 2 * KC:3 * KC])
        if stop_after == f"mrg{li}":
            break
        seqs = ([] if last else [("c", cres, LC, 1, chx2, cmoe)]) + [("x", xres, L, 0, hx2, xmoe)]
        for nm, res_, L_, r_, hx_, mo_ in seqs:
            cap_ = 2 * L_ // c.E
            SLT_ = min(128, cap_)
            with ExitStack() as es2:
                idxTi = es2.enter_context(nc.sbuf_tensor(f"idxTi{nm}{li}", [128, cap_ // SLT_, c.E], I32))
                gatesT = es2.enter_context(nc.sbuf_tensor(f"gatesT{nm}{li}", [128, cap_ // SLT_, c.E], F32))
                with ExitStack() as es3:
                    affT = es3.enter_context(nc.sbuf_tensor(f"affT{nm}{li}", [c.E, L_], F32))
                    phase_moe_a(k, f"{nm}a{li}", res_, L_, AB[:, r_, 1, :], modt[:, r_, 3 * KC:4 * KC], w_router_in, li, consts, hx_, affT)
                    phase_moe_b(k, f"{nm}b{li}", L_, affT, consts, idxTi, gatesT)
                phase_moe_c(k, f"{nm}c{li}", L_, li, W, hx_, idxTi, gatesT, mo_, consts)
                phase_moe_d(k, f"{nm}d{li}", L_, mo_, res_, modt[:, r_, 5 * KC:6 * KC], consts)
        if stop_after == f"moe{li}":
            break

    if stop_after is None:
        phase_final(k, "fin", xres, L, gfin[:, 0, :], consts, outT)
    else:
        fs = k.sem("fin")
        sig(k.sp.dma_start(out=outT[:, :], in_=xres[:, :]), fs, 16)
        k.sp.wait_ge(fs.h, fs.v)
    return nc, dbg


def _seq_dft(n):
    t = np.arange(n, dtype=np.int64)
    ph = (np.outer(t, t) % n).astype(np.float64) * (2.0 * np.pi / n)
    m = np.concatenate([np.cos(ph), -np.sin(ph)], axis=1) / np.sqrt(n)
    return np.ascontiguousarray(m.astype(np.float32).astype(ml_dtypes.bfloat16))


def _chan_dft(c):
    gd = c.GD
    a = np.arange(gd, dtype=np.int64)
    ph = (np.outer(a, a) % gd).astype(np.float64) * (2.0 * np.pi / gd)
    m = np.zeros((c.DF, 2 * c.DF), np.float64)
    for g in range(c.DF // gd):
        m[g * gd:(g + 1) * gd, g * gd:(g + 1) * gd] = np.cos(ph) / np.sqrt(gd)
        m[g * gd:(g + 1) * gd, c.DF + g * gd:c.DF + (g + 1) * gd] = np.sin(ph) / np.sqrt(gd)
    return np.ascontiguousarray(m.astype(np.float32).astype(ml_dtypes.bfloat16))


def _rowcol(c):
    t = np.arange(c.L)
    rc = np.stack([(t // c.GRID_W).astype(np.float32), (t % c.GRID_W).astype(np.float32)], 0)
    return np.ascontiguousarray(np.broadcast_to(rc[None], (128, 2, c.L)).astype(np.float32))


def _invf(c):
    q = c.D // 4
    f = (10000.0 ** (-np.arange(q, dtype=np.float32) / np.float32(q))).astype(np.float32)
    return np.ascontiguousarray(np.tile(f, 4).reshape(1, c.D))


def prepare_in_maps(cfg, inp):
    c = cfg
    f32 = np.float32
    maps = []
    big = {}
    for li in range(c.DEPTH):
        for name, rf, cf in BIG_W:
            big[(name, li)] = np.ascontiguousarray(np.asarray(inp[name][li], dtype=f32).reshape(rf(c), cf(c)))
    common = {
        "ada_b": np.ascontiguousarray(np.asarray(inp["ada_b"], f32)),
        "norm_mix_g": np.ascontiguousarray(np.asarray(inp["norm_mix_g"], f32)),
        "norm_ffn_g": np.ascontiguousarray(np.asarray(inp["norm_ffn_g"], f32)),
        "norm_final_g": np.ascontiguousarray(np.asarray(inp["norm_final_g"], f32).reshape(1, c.D)),
        "ident32": np.eye(128, dtype=f32),
        "iota512": np.ascontiguousarray(np.broadcast_to(np.arange(512, dtype=f32)[None, :], (128, 512))),
        "s5_lam_re": np.ascontiguousarray(np.asarray(inp["s5_lam_re"], f32)),
        "s5_lam_im": np.ascontiguousarray(np.asarray(inp["s5_lam_im"], f32)),
        "s5_log_dt": np.ascontiguousarray(np.asarray(inp["s5_log_dt"], f32)),
        "s5_b_re": np.ascontiguousarray(np.asarray(inp["s5_b_re"], f32)),
        "s5_b_im": np.ascontiguousarray(np.asarray(inp["s5_b_im"], f32)),
        "s5_c_re": np.ascontiguousarray(np.asarray(inp["s5_c_re"], f32)),
        "s5_c_im": np.ascontiguousarray(np.asarray(inp["s5_c_im"], f32)),
        "s5_d": np.ascontiguousarray(np.asarray(inp["s5_d"], f32)),
        "CD": _chan_dft(c),
        "rowcol": _rowcol(c),
        "invf": _invf(c),
        "w_router": np.ascontiguousarray(np.asarray(inp["w_router"], f32)),
        "DLc": _seq_dft(c.LC),
    }
    dlx = _seq_dft(c.L)
    x = np.asarray(inp["x"], f32)
    ctx = np.asarray(inp["ctx"], f32)
    cc = np.asarray(inp["c"], f32)
    c_ctx = np.asarray(inp["c_ctx"], f32)
    nb = x.shape[0]
    for core in range(NCORES):
        b = core % nb
        m = dict(common)
        m["xT"] = np.ascontiguousarray(x[b].T)
        m["cxT"] = np.ascontiguousarray(ctx[b].T)
        m["cvec"] = np.ascontiguousarray(np.stack([cc[b], c_ctx], 0))
        for (name, li), w in big.items():
            rs = w.shape[0] // NCORES
            m[f"{name}{li}"] = w[core * rs:(core + 1) * rs]
        rs = c.L // NCORES
        m["DLx"] = dlx[core * rs:(core + 1) * rs]
        maps.append(m)
    return maps


def bc_last(ap2d, n):
    a = ap2d.ap
    return AP(ap2d.tensor, ap2d.offset, [list(a[0]), list(a[1]), [0, n]])


def emit_range_reduce(dve, x, tf, ti, ab, fn=None):
    fn = fn or (lambda i: i)
    fn(dve.tensor_scalar(out=tf, in0=x, scalar1=1.0 / TWO_PI, scalar2=None, op0=ALU.mult))
    fn(dve.tensor_copy(out=ti, in_=tf))
    fn(dve.tensor_copy(out=tf, in_=ti))
    fn(dve.scalar_tensor_tensor(out=x, in0=tf, scalar=-TWO_PI, in1=x, op0=ALU.mult, op1=ALU.add))
    fn(dve.tensor_scalar(out=x, in0=x, scalar1=3.14159, scalar2=-3.14159, op0=ALU.min, op1=ALU.max))
    return fn(dve.scalar_tensor_tensor(out=ab, in0=x, scalar=-1.0, in1=x, op0=ALU.mult, op1=ALU.max))


def s5_setup(k, li, prm, S):
    nc, c = k.nc, k.cfg
    NGP, G, P_, N_ = c.NGP, c.G, c.P, c.N
    consts = S["consts"]
    with ExitStack() as es:
        def sb(name, shape, dt=F32):
            return es.enter_context(nc.sbuf_tensor(f"s5s{li}_{name}", shape, dt))
        ld = k.sem("s5ld")
        pr = PsRing(k, "s5sp", [1, 2, 3, 4])
        for d in range(2):
            LR, LI, LDT = sb(f"lr{d}", [128, NGP]), sb(f"li{d}", [128, NGP]), sb(f"ldt{d}", [128, NGP])
            Bn = [sb(f"bn{d}{r}", [128, NGP, N_]) for r in range(2)]
            Bb = [sb(f"bb{d}{r}", [128, NGP, N_]) for r in range(2)]
            Tb = sb(f"tb{d}", [128, NGP, 128])
            Cb = [sb(f"cb{d}{r}", [128, NGP, 128]) for r in range(2)]
            t = [sb(f"t{d}{i}", [128, NGP]) for i in range(8)]
            tw = [sb(f"tw{d}{i}", [128, NGP, N_]) for i in range(2)]
            tI = sb(f"tI{d}", [128, NGP], I32)
            k.pool.memset(Cb[0][:, :, :], 0.0)
            k.pool.memset(Cb[1][:, :, :], 0.0)
            sig(k.pool.memset(Tb[:, :, :], 0.0), ld)
            k.sp.wait_ge(ld.h, ld.v)
            base = (li * 2 + d)
            with nc.allow_non_contiguous_dma(reason="small s5 param layouts"):
                for name, dst in (("lam_re", LR), ("lam_im", LI)):
                    src = prm[name].ap()[li, d].rearrange("g p -> (g p)").rearrange("(gp q) -> q gp", q=128)
                    sig(k.sp.dma_start(out=dst[:, :], in_=src), ld, 16)
                ldt_t = prm["log_dt"]
                for g2 in range(2):
                    src = AP(ldt_t.ap().tensor, base * G + g2, [[0, 64], [2, NGP]])
                    sig(k.sp.dma_start(out=LDT[g2 * 64:(g2 + 1) * 64, :], in_=src), ld, 16)
                for r, name in enumerate(("b_re", "b_im")):
                    src = prm[name].ap()[li, d].rearrange("g p n -> (g p n)").rearrange("(gp q n) -> q gp n", q=128, n=N_)
                    sig(k.sp.dma_start(out=Bn[r][:, :, :], in_=src), ld, 16)
                for r, name in enumerate(("c_re", "c_im")):
                    ct = prm[name]
                    for g8 in range(8):
                        src = AP(ct.ap().tensor, (base * G + g8) * N_ * P_, [[P_, N_], [8 * N_ * P_, c.SC], [1, P_]])
                        dst = Cb[r][16 * g8:16 * g8 + 16, (g8 // 2)::4, (g8 % 2) * 64:(g8 % 2) * 64 + 64]
                        sig(k.sp.dma_start(out=dst, in_=src), ld, 16)
            k.dve.wait_ge(ld.h, ld.v)
            k.act.wait_ge(ld.h, ld.v)
            k.pe.wait_ge(ld.h, ld.v)
            sc = S["sc"]

            class _Fenced:
                def __init__(self, eng):
                    self._e = eng

                def wait_ge(self, *a, **kw):
                    return self._e.wait_ge(*a, **kw)

                def __getattr__(self, name):
                    f = getattr(self._e, name)

                    def g(*a, **kw):
                        return k.fence(self._e, f(*a, **kw))
                    return g
            dv, ac = _Fenced(k.dve), _Fenced(k.act)
            s_da = k.sem("s5da")
            s_ad = k.sem("s5ad")
            dv.tensor_scalar(out=LR[:, :], in0=LR[:, :], scalar1=-1e-4, scalar2=None, op0=ALU.min)
            ac.activation(out=LDT[:, :], in_=LDT[:, :], func=AF.Exp)
            sig(ac.activation(out=t[7][:, :], in_=LDT[:, :], func=AF.Copy), s_ad)
            dv.wait_ge(s_ad.h, s_ad.v)
            dv.tensor_tensor(out=t[0][:, :], in0=LR[:, :], in1=LDT[:, :], op=ALU.mult)
            dv.tensor_tensor(out=sc[:, d, 1, :], in0=LI[:, :], in1=LDT[:, :], op=ALU.mult)
            dv.tensor_copy(out=t[1][:, :], in_=sc[:, d, 1, :])
            sig(emit_range_reduce(dv, t[1][:, :], t[3][:, :], tI[:, :], t[2][:, :]), s_da)
            ac.wait_ge(s_da.h, s_da.v)
            ac.activation(out=sc[:, d, 0, :], in_=t[0][:, :], func=AF.Exp)
            ac.activation(out=sc[:, d, 3, :], in_=t[1][:, :], func=AF.Sin)
            sig(ac.activation(out=sc[:, d, 2, :], in_=t[2][:, :], func=AF.Sin, scale=-1.0, bias=consts["hpi"][:, 0:1]), s_ad)
            dv.wait_ge(s_ad.h, s_ad.v)
            dv.tensor_tensor(out=sc[:, d, 4, :], in0=sc[:, d, 0, :], in1=sc[:, d, 2, :], op=ALU.mult)
            dv.tensor_tensor(out=sc[:, d, 5, :], in0=sc[:, d, 0, :], in1=sc[:, d, 3, :], op=ALU.mult)
            dv.tensor_scalar(out=sc[:, d, 6, :], in0=sc[:, d, 3, :], scalar1=-1.0, scalar2=None, op0=ALU.mult)
            dv.tensor_scalar(out=t[0][:, :], in0=sc[:, d, 4, :], scalar1=-1.0, scalar2=None, op0=ALU.add)
            dv.tensor_tensor(out=t[1][:, :], in0=LR[:, :], in1=LR[:, :], op=ALU.mult)
            dv.tensor_tensor(out=t[2][:, :], in0=LI[:, :], in1=LI[:, :], op=ALU.mult)
            dv.tensor_tensor(out=t[1][:, :], in0=t[1][:, :], in1=t[2][:, :], op=ALU.add)
            dv.reciprocal(out=t[1][:, :], in_=t[1][:, :])
            dv.tensor_tensor(out=t[2][:, :], in0=t[0][:, :], in1=LR[:, :], op=ALU.mult)
            dv.tensor_tensor(out=t[3][:, :], in0=sc[:, d, 5, :], in1=LI[:, :], op=ALU.mult)
            dv.tensor_tensor(out=t[2][:, :], in0=t[2][:, :], in1=t[3][:, :], op=ALU.add)
            dv.tensor_tensor(out=t[4][:, :], in0=t[2][:, :], in1=t[1][:, :], op=ALU.mult)
            dv.tensor_tensor(out=t[2][:, :], in0=sc[:, d, 5, :], in1=LR[:, :], op=ALU.mult)
            dv.tensor_tensor(out=t[3][:, :], in0=t[0][:, :], in1=LI[:, :], op=ALU.mult)
            dv.tensor_tensor(out=t[2][:, :], in0=t[2][:, :], in1=t[3][:, :], op=ALU.subtract)
            dv.tensor_tensor(out=t[5][:, :], in0=t[2][:, :], in1=t[1][:, :], op=ALU.mult)
            fre, fim = bc_last(t[4][:, :], N_), bc_last(t[5][:, :], N_)
            dv.tensor_tensor(out=tw[0][:, :, :], in0=Bn[0][:, :, :], in1=fre, op=ALU.mult)
            dv.tensor_tensor(out=tw[1][:, :, :], in0=Bn[1][:, :, :], in1=fim, op=ALU.mult)
            dv.tensor_tensor(out=Bb[0][:, :, :], in0=tw[0][:, :, :], in1=tw[1][:, :, :], op=ALU.subtract)
            dv.tensor_tensor(out=tw[0][:, :, :], in0=Bn[1][:, :, :], in1=fre, op=ALU.mult)
            dv.tensor_tensor(out=tw[1][:, :, :], in0=Bn[0][:, :, :], in1=fim, op=ALU.mult)
            dv.tensor_tensor(out=Bb[1][:, :, :], in0=tw[0][:, :, :], in1=tw[1][:, :, :], op=ALU.add)
            ev = 0
            for r in range(2):
                lastp = None
                for q in range(4):
                    for g2 in range(2):
                        lastp = dv.tensor_copy(out=Tb[g2 * 64:(g2 + 1) * 64, q::4, (2 * q + g2) * 16:(2 * q + g2) * 16 + 16],
                                               in_=Bb[r][g2 * 64:(g2 + 1) * 64, q::4, :])
                s_tp = k.sem("s5tp")
                sig(lastp, s_tp)
                k.pe.wait_ge(s_tp.h, s_tp.v)
                lastc = None
                for gp in range(NGP):
                    ps = pr.wslot(k)
                    pr.wdone(k.pe.transpose(out=ps[:, 0:128], in_=Tb[:, gp, :], identity=consts["ident32"][:, :]))
                    eng = k.dve if ev % 2 == 0 else k.act
                    ev += 1
                    ps = pr.rslot(k, eng)
                    if eng is k.dve:
                        lastc = eng.tensor_copy(out=S["lB"][d][r][:, gp, :], in_=ps[:, 0:128])
                    else:
                        lastc = eng.activation(out=S["lB"][d][r][:, gp, :], in_=ps[:, 0:128], func=AF.Copy)
                    pr.rdone(lastc)
                s_w = k.sem("s5w")
                sig(lastc, s_w)
                dv.wait_ge(s_w.h, s_w.v)
                if ev % 2 == 0:
                    pass
            for r in range(2):
                for gp in range(NGP):
                    ps = pr.wslot(k)
                    pr.wdone(k.pe.transpose(out=ps[:, 0:128], in_=Cb[r][:, gp, :], identity=consts["ident32"][:, :]))
                    ps = pr.rslot(k, k.act)
                    a = k.act.activation(out=S["lR"][d][r][:, gp, :], in_=ps[:, 0:128], func=AF.Copy,
                                         scale=(1.0 if r == 0 else -1.0))
                    pr.rdone(a)
    k.barrier()


def phase_s5(k, tag, li, uT, L, S, yT, do_readout, dcols):
    nc, c = k.nc, k.cfg
    NGP, SC = c.NGP, c.SC
    TC = min(512, L)
    NT = L // TC
    consts = S["consts"]
    sc, hcar = S["sc"], S["hcar"]
    pe, act, dve, pool, sp = k.pe, k.act, k.dve, k.pool, k.sp
    with ExitStack() as es:
        def sb(name, shape, dt=F32):
            return es.enter_context(nc.sbuf_tensor(f"{tag}_{name}", shape, dt))
        ut = [sb(f"u{i}", [128, L], BF16) for i in range(2)]
        uring = Ring(k, tag + "u", 2)
        psbu = Ring(k, tag + "pb", 2)
        bu_banks = [(1, 2), (3, 4)]
        bt = [(sb(f"br{i}", [128, TC]), sb(f"bi{i}", [128, TC])) for i in range(2)]
        bring = Ring(k, tag + "b", 2)
        vt = [(sb(f"vr{i}", [128, TC]), sb(f"vi{i}", [128, TC])) for i in range(2)]
        vring = Ring(k, tag + "v", 2)
        ht = [(sb(f"hr{i}", [128, TC], BF16), sb(f"hi{i}", [128, TC], BF16)) for i in range(2)]
        hring = Ring(k, tag + "h", 2)
        psy = PsRing(k, tag + "py", [5, 6])
        tabs = [[(sb(f"cos{s}{q}", [128, TC]), sb(f"sin{s}{q}", [128, TC])) for q in range(4)] for s in range(2)]
        tring = Ring(k, tag + "t", 2)
        ang = [sb(f"ang{i}", [128, TC]) for i in range(2)]
        aab = [sb(f"aab{i}", [128, TC]) for i in range(2)]
        angf = sb("angf", [128, TC])
        angi = sb("angi", [128, TC], I32)
        aring = Ring(k, tag + "a", 2)
        pt = [sb(f"pt{i}", [128, TC]) for i in range(2)]
        gre, gim, d1, d2 = sb("gre", [128, TC]), sb("gim", [128, TC]), sb("d1", [128, TC]), sb("d2", [128, TC])
        tiny = sb("tiny", [128, 8])
        yacc = sb("yacc", [128, L])
        ysem_f = k.sem(tag + "yf")
        ysem_e = k.sem(tag + "ye")
        ye_need = 0
        et = [sb(f"et{i}", [128, TC]) for i in range(3)]
        st = OutStage(k, es, tag + "o", [128, TC], BF16, 2)
        ering = Ring(k, tag + "er", 1)
        s_g = k.sem(tag + "g1")
        s_g2 = k.sem(tag + "g2")
        iota = consts["iota"]

        def rev(ap2d):
            a = ap2d.ap
            n = a[1][1]
            return AP(ap2d.tensor, ap2d.offset + (n - 1) * a[1][0], [list(a[0]), [-a[1][0], n]])

        for cch in range(SC):
            us = uring.wslot(sp)
            uring.wdone(sp.dma_start(out=ut[us][:, :], in_=uT.ap()[cch * 128:(cch + 1) * 128, :]), 16)
            uring.wcommit()
            us = uring.rslot(pe, dve)
            u = ut[us]
            last_pe = None
            last_dve_u = None
            for d in range(2):
                ts_ = tring.wslot(dve, act)
                for q in range(4):
                    gp = 4 * cch + q
                    a_s = aring.wslot(dve)
                    dve.tensor_scalar(out=ang[a_s][:, :], in0=iota[:, 0:TC], scalar1=sc[:, d, 1, gp:gp + 1], scalar2=None, op0=ALU.mult)
                    i1 = emit_range_reduce(dve, ang[a_s][:, :], angf[:, :], angi[:, :], aab[a_s][:, :])
                    aring.wdone(i1)
                    aring.wcommit()
                    a_s = aring.rslot(act)
                    act.activation(out=tabs[ts_][q][1][:, :], in_=ang[a_s][:, :], func=AF.Sin)
                    a = act.activation(out=tabs[ts_][q][0][:, :], in_=aab[a_s][:, :], func=AF.Sin, scale=-1.0, bias=consts["hpi"][:, 0:1])
                    aring.rdone(a)
                tring.wdone(a)
                tring.wcommit()
                ts_ = tring.rslot(pool, dve)
                tcs = range(NT) if d == 0 else range(NT - 1, -1, -1)
                last_pool = None
                last_dve = None
                for tc in tcs:
                    t0 = tc * TC
                    for q in range(4):
                        gp = 4 * cch + q
                        cosT, sinT = tabs[ts_][q]
                        pb = psbu.wslot(pe)
                        b_re, b_im = k.ps[bu_banks[pb][0]], k.ps[bu_banks[pb][1]]
                        pe.matmul(b_re[:, 0:TC], lhsT=S["lB"][d][0][:, gp, :], rhs=u[:, t0:t0 + TC], start=True, stop=True)
                        psbu.wdone(pe.matmul(b_im[:, 0:TC], lhsT=S["lB"][d][1][:, gp, :], rhs=u[:, t0:t0 + TC], start=True, stop=True))
                        psbu.wcommit()
                        pb = psbu.rslot(act)
                        bs = bring.wslot(act)
                        br, bi = bt[bs]
                        o_re = br[:, :] if d == 0 else rev(br[:, :])
                        o_im = bi[:, :] if d == 0 else rev(bi[:, :])
                        act.activation(out=o_re, in_=k.ps[bu_banks[pb][0]][:, 0:TC], func=AF.Copy)
                        a = act.activation(out=o_im, in_=k.ps[bu_banks[pb][1]][:, 0:TC], func=AF.Copy)
                        psbu.rdone(a)
                        bring.wdone(a)
                        bring.wcommit()
                        bs = bring.rslot(pool)
                        br, bi = bt[bs]
                        vs = vring.wslot(pool)
                        vr, vi = vt[vs]
                        pool.tensor_tensor(out=pt[0][:, :], in0=br[:, :], in1=cosT[:, :], op=ALU.mult)
                        pool.tensor_tensor(out=pt[1][:, :], in0=bi[:, :], in1=sinT[:, :], op=ALU.mult)
                        pool.tensor_tensor(out=vr[:, :], in0=pt[0][:, :], in1=pt[1][:, :], op=ALU.add)
                        pool.tensor_tensor(out=pt[0][:, :], in0=bi[:, :], in1=cosT[:, :], op=ALU.mult)
                        pool.tensor_tensor(out=pt[1][:, :], in0=br[:, :], in1=sinT[:, :], op=ALU.mult)
                        last_pool = pool.tensor_tensor(out=vi[:, :], in0=pt[0][:, :], in1=pt[1][:, :], op=ALU.subtract)
                        bring.rdone(last_pool)
                        vring.wdone(last_pool)
                        vring.wcommit()
                        vs = vring.rslot(dve)
                        vr, vi = vt[vs]
                        hp = hcar[:, d, gp, :]
                        cth, sth, nsth, magc = (sc[:, d, 2, gp:gp + 1], sc[:, d, 3, gp:gp + 1], sc[:, d, 6, gp:gp + 1],
                                                sc[:, d, 0, gp:gp + 1])
                        dve.tensor_tensor(out=tiny[:, 0:1], in0=hp[:, 0:1], in1=cth, op=ALU.mult)
                        k.fence(dve, dve.tensor_tensor(out=tiny[:, 2:3], in0=hp[:, 0:1], in1=sth, op=ALU.mult))
                        dve.scalar_tensor_tensor(out=tiny[:, 1:2], in0=hp[:, 1:2], scalar=nsth, in1=tiny[:, 0:1], op0=ALU.mult, op1=ALU.add)
                        k.fence(dve, dve.scalar_tensor_tensor(out=tiny[:, 3:4], in0=hp[:, 1:2], scalar=cth, in1=tiny[:, 2:3], op0=ALU.mult, op1=ALU.add))
                        magb = magc.broadcast_to([128, TC])
                        dve.tensor_tensor_scan(out=gre[:, :], data0=magb, data1=vr[:, :], initial=tiny[:, 1:2], op0=ALU.mult, op1=ALU.add)
                        i2 = dve.tensor_tensor_scan(out=gim[:, :], data0=magb, data1=vi[:, :], initial=tiny[:, 3:4], op0=ALU.mult, op1=ALU.add)
                        vring.rdone(i2)
                        hs = hring.wslot(dve)
                        hr, hi = ht[hs]
                        o_hr = hr[:, :] if d == 0 else rev(hr[:, :])
                        o_hi = hi[:, :] if d == 0 else rev(hi[:, :])
                        dve.tensor_tensor(out=d1[:, :], in0=gre[:, :], in1=cosT[:, :], op=ALU.mult)
                        dve.tensor_tensor(out=d2[:, :], in0=gim[:, :], in1=sinT[:, :], op=ALU.mult)
                        k.fence(dve, dve.tensor_tensor(out=o_hr, in0=d1[:, :], in1=d2[:, :], op=ALU.subtract))
                        k.fence(dve, dve.tensor_tensor(out=hp[:, 0:1], in0=d1[:, TC - 1:TC], in1=d2[:, TC - 1:TC], op=ALU.subtract))
                        dve.tensor_tensor(out=d1[:, :], in0=gre[:, :], in1=sinT[:, :], op=ALU.mult)
                        dve.tensor_tensor(out=d2[:, :], in0=gim[:, :], in1=cosT[:, :], op=ALU.mult)
                        last_dve = dve.tensor_tensor(out=o_hi, in0=d1[:, :], in1=d2[:, :], op=ALU.add)
                        k.fence(dve, dve.tensor_tensor(out=hp[:, 1:2], in0=d1[:, TC - 1:TC], in1=d2[:, TC - 1:TC], op=ALU.add))
                        hring.wdone(last_dve)
                        hring.wcommit()
                        hs = hring.rslot(pe)
                        if do_readout:
                            hr, hi = ht[hs]
                            if q == 0:
                                py = psy.wslot(k)
                            pe.matmul(py[:, 0:TC], lhsT=S["lR"][d][0][:, gp, :], rhs=hr[:, :], start=(q == 0), stop=False)
                            last_pe = pe.matmul(py[:, 0:TC], lhsT=S["lR"][d][1][:, gp, :], rhs=hi[:, :], start=False, stop=(q == 3))
                            hring.rdone(last_pe)
                        else:
                            hring.rdone(last_dve)
                    if not do_readout:
                        continue
                    psy.wdone(last_pe)
                    if d == 0:
                        py = psy.rslot(k, act)
                        if ye_need and tc == 0:
                            act.wait_ge(ysem_e.h, ye_need)
                        a = act.activation(out=yacc[:, t0:t0 + TC], in_=py[:, 0:TC], func=AF.Copy)
                        psy.rdone(a)
                        if tc == NT - 1:
                            sig(a, ysem_f)
                    else:
                        py = psy.rslot(k, dve)
                        if tc == NT - 1:
                            dve.wait_ge(ysem_f.h, ysem_f.v)
                        er = ering.wslot(dve)
                        dve.tensor_tensor(out=et[0][:, :], in0=py[:, 0:TC], in1=yacc[:, t0:t0 + TC], op=ALU.add)
                        i3 = dve.scalar_tensor_tensor(out=et[0][:, :], in0=u[:, t0:t0 + TC], scalar=dcols[:, cch:cch + 1],
                                                      in1=et[0][:, :], op0=ALU.mult, op1=ALU.add)
                        psy.rdone(i3)
                        last_dve_u = i3
                        if tc == 0:
                            sig(i3, ysem_e)
                            ye_need = ysem_e.v
                        ering.wdone(i3)
                        ering.wcommit()
                        ering.rslot(pool)
                        pool.tensor_tensor(out=et[1][:, :], in0=et[0][:, :], in1=et[0][:, :], op=ALU.mult)
                        pool.tensor_scalar(out=et[1][:, :], in0=et[1][:, :], scalar1=0.044715, scalar2=1.0, op0=ALU.mult, op1=ALU.add)
                        sig(pool.tensor_tensor(out=et[1][:, :], in0=et[1][:, :], in1=et[0][:, :], op=ALU.mult), s_g)
                        act.wait_ge(s_g.h, s_g.v)
                        sig(act.activation(out=et[2][:, :], in_=et[1][:, :], func=AF.Sigmoid, scale=1.5957691216057308), s_g2)
                        pool.wait_ge(s_g2.h, s_g2.v)
                        o = st.wslot(pool)
                        i4 = pool.tensor_tensor(out=o[:, :], in0=et[0][:, :], in1=et[2][:, :], op=ALU.mult)
                        ering.rdone(i4)
                        st.store(k, i4, yT.ap()[cch * 128:(cch + 1) * 128, t0:t0 + TC])
                tring.rdone(last_pool)
                tring.rdone(last_dve)
            uring.rdone(last_pe if do_readout else pe.matmul(k.ps[7][0:8, 0:8], lhsT=k.scrb[:, 0:8], rhs=k.scrb[:, 0:8], start=True, stop=True))
            if last_dve_u is not None:
                uring.rdone(last_dve_u)
        st.drain(k)
    k.barrier()


def phase_fft(k, tag, vT, L, CD, DL, yfT):
    nc, c = k.nc, k.cfg
    pe, act, dve, sp = k.pe, k.act, k.dve, k.sp
    DF = c.DF
    FCn = DF // 128
    gspan = max(c.GD, 128) // 128
    KB = min(DF, 512)
    KBC = KB // 128
    LT = L // 128
    TW = min(256, L)
    with ExitStack() as es:
        def sb(name, shape, dt=BF16):
            return es.enter_context(nc.sbuf_tensor(f"{tag}_{name}", shape, dt))
        vt = sb("v", [128, KBC, L])
        cd = sb("cd", [128, KBC, 2, KB])
        VC = sb("VC", [128, LT, 2, KB])
        dl = [sb(f"dl{i}", [128, LT, 2, TW]) for i in range(2)]
        dring = Ring(k, tag + "d", 2)
        pr = PsRing(k, tag + "p", [1, 2, 3, 4])
        st = OutStage(k, es, tag + "o", [128, TW], BF16, 3)
        ld = k.sem(tag + "ld")
        vfree = k.sem(tag + "vf")
        vneed = 0
        dlv = DL.ap().rearrange("(lt p) c -> p lt c", p=128)
        ev = 0
        for pb in range(DF // KB):
            if vneed:
                sp.wait_ge(vfree.h, vneed)
            for kk in range(KBC):
                ki = pb * KBC + kk
                sig(sp.dma_start(out=vt[:, kk, :], in_=vT.ap()[ki * 128:(ki + 1) * 128, :]), ld, 16)
                for cs in range(2):
                    sig(sp.dma_start(out=cd[:, kk, cs, :], in_=CD.ap()[ki * 128:(ki + 1) * 128, cs * DF + pb * KB: cs * DF + (pb + 1) * KB]), ld, 16)
            pe.wait_ge(ld.h, ld.v)
            last = None
            for lt in range(LT):
                for cs in range(2):
                    ps = pr.wslot(k)
                    for kol in range(KBC):
                        kis = [kk for kk in range(KBC) if (pb * KBC + kk) // gspan == (pb * KBC + kol) // gspan]
                        for n_, kk in enumerate(kis):
                            last = pe.matmul(ps[:, kol * 128:(kol + 1) * 128], lhsT=vt[:, kk, lt * 128:(lt + 1) * 128],
                                             rhs=cd[:, kk, cs, kol * 128:(kol + 1) * 128], start=(n_ == 0), stop=(n_ == len(kis) - 1))
                    pr.wdone(last)
                    eng = dve if ev % 2 == 0 else act
                    ev += 1
                    ps = pr.rslot(k, eng)
                    if eng is dve:
                        ins = dve.tensor_copy(out=VC[:, lt, cs, :], in_=ps[:, 0:KB])
                    else:
                        ins = act.activation(out=VC[:, lt, cs, :], in_=ps[:, 0:KB], func=AF.Copy)
                    pr.rdone(ins)
            sig(last, vfree)
            vneed = vfree.v
            vs = k.sem(tag + "vc")
            sig(dve.memset(k.scr[:, 0:1], 0.0), vs)
            sig(act.activation(out=k.scr[:, 2:3], in_=k.scr[:, 3:4], func=AF.Copy), vs)
            pe.wait_ge(vs.h, vs.v)
            for tb in range(L // TW):
                s = dring.wslot(sp)
                for cs in range(2):
                    dring.wdone(sp.dma_start(out=dl[s][:, :, cs, :], in_=dlv[:, :, cs * L + tb * TW: cs * L + (tb + 1) * TW]), 16)
                dring.wcommit()
                s = dring.rslot(pe)
                for kol in range(KBC):
                    ps = pr.wslot(k)
                    n_ = 0
                    for lt in range(LT):
                        for cs in range(2):
                            last = pe.matmul(ps[:, 0:TW], lhsT=VC[:, lt, cs, kol * 128:(kol + 1) * 128], rhs=dl[s][:, lt, cs, :],
                                             start=(n_ == 0), stop=(n_ == 2 * LT - 1))
                            n_ += 1
                    pr.wdone(last)
                    eng = dve if ev % 2 == 0 else act
                    ev += 1
                    ps = pr.rslot(k, eng)
                    o = st.wslot(eng)
                    if eng is dve:
                        ins = dve.tensor_copy(out=o[:, :], in_=ps[:, 0:TW])
                    else:
                        ins = act.activation(out=o[:, :], in_=ps[:, 0:TW], func=AF.Copy)
                    pr.rdone(ins)
                    ko = pb * KBC + kol
                    st.store(k, ins, yfT.ap()[ko * 128:(ko + 1) * 128, tb * TW:(tb + 1) * TW])
                dring.rdone(last)
            vw = k.sem(tag + "vw")
            sig(last, vw)
            dve.wait_ge(vw.h, vw.v)
            act.wait_ge(vw.h, vw.v)
        st.drain(k)
    k.barrier()


def phase_merge(k, tag, L, yT, yfT, g1T, g2T, xres, Wd, li, gcol):
    nc, c = k.nc, k.cfg
    pe, act, dve, pool, sp = k.pe, k.act, k.dve, k.pool, k.sp
    KC, SC, FCn = c.KC, c.SC, c.DF // 128
    TB = min(512, L)
    w_glu, w_s5o, w_fto, w_out = Wd[("w_glu", li)], Wd[("w_s5_out", li)], Wd[("w_ft_out", li)], Wd[("w_out", li)]
    with ExitStack() as es:
        def sb(name, shape, dt=BF16):
            return es.enter_context(nc.sbuf_tensor(f"{tag}_{name}", shape, dt))
        yb, yfb, ysb, mT = sb("yb", [128, SC, TB]), sb("yfb", [128, FCn, TB]), sb("ysb", [128, SC, TB]), sb("mT", [128, KC, TB])
        GWg = min(512, c.DS)
        lw_g = LinW(k, es, tag + "wg", SC, GWg, 2)
        lw_s = LinW(k, es, tag + "ws", SC, 512, 2)
        lw_f = LinW(k, es, tag + "wf", FCn, 512, 2)
        lw_o = LinW(k, es, tag + "wo", KC, 256, 2)
        pr = PsRing(k, tag + "p", [1, 2, 3, 4, 5, 6])
        sgt = [sb(f"sg{i}", [128, TB]) for i in range(2)]
        sgr = Ring(k, tag + "sg", 2)
        g1t = [sb(f"g1{i}", [128, TB]) for i in range(2)]
        g2t = [sb(f"g2{i}", [128, TB]) for i in range(2)]
        gr = Ring(k, tag + "g", 2)
        t1 = [sb(f"t1{i}", [128, TB], F32) for i in range(2)]
        t2 = [sb(f"t2{i}", [128, TB], F32) for i in range(2)]
        tr = Ring(k, tag + "t", 2)
        xt = [sb(f"x{i}", [128, TB], F32) for i in range(2)]
        xr = Ring(k, tag + "x", 2)
        st = OutStage(k, es, tag + "o", [128, TB], F32, 3)
        ld = k.sem(tag + "ld")
        yfree = k.sem(tag + "yf")
        yneed = 0
        ms = k.sem(tag + "ms")
        mfree = k.sem(tag + "mf")
        mneed = 0
        ysd = k.sem(tag + "ys")
        for tb in range(L // TB):
            t0 = tb * TB
            if yneed:
                sp.wait_ge(yfree.h, yneed)
            for kc in range(SC):
                sig(sp.dma_start(out=yb[:, kc, :], in_=yT.ap()[kc * 128:(kc + 1) * 128, t0:t0 + TB]), ld, 16)
            for kc in range(FCn):
                sig(sp.dma_start(out=yfb[:, kc, :], in_=yfT.ap()[kc * 128:(kc + 1) * 128, t0:t0 + TB]), ld, 16)
            pe.wait_ge(ld.h, ld.v)
            dve.wait_ge(ld.h, ld.v)
            last = None
            lastd = None
            for jg in range(0, SC, GWg // 128):
                lw_g.load(k, w_glu, 0, jg * 128)
                wt = lw_g.get(k)
                for jj in range(GWg // 128):
                    jo = jg + jj
                    ps = pr.wslot(k)
                    for kc in range(SC):
                        last = pe.matmul(ps[:, 0:TB], lhsT=wt[:, kc, jj * 128:(jj + 1) * 128], rhs=yb[:, kc, :], start=(kc == 0), stop=(kc == SC - 1))
                    pr.wdone(last)
                    ps = pr.rslot(k, act)
                    s = sgr.wslot(act)
                    a = act.activation(out=sgt[s][:, :], in_=ps[:, 0:TB], func=AF.Sigmoid)
                    pr.rdone(a)
                    sgr.wdone(a)
                    sgr.wcommit()
                    s = sgr.rslot(dve)
                    lastd = dve.tensor_tensor(out=ysb[:, jo, :], in0=yb[:, jo, :], in1=sgt[s][:, :], op=ALU.mult)
                    sgr.rdone(lastd)
                lw_g.done(last)
            sig(lastd, ysd)
            pe.wait_ge(ysd.h, ysd.v)
            if mneed:
                pool.wait_ge(mfree.h, mneed)
            lastp = None
            for jg in range(0, KC, 4):
                lw_s.load(k, w_s5o, 0, jg * 128)
                lw_f.load(k, w_fto, 0, jg * 128)
                ws, wf = lw_s.get(k), lw_f.get(k)
                for jj in range(4):
                    jo = jg + jj
                    gs = gr.wslot(sp)
                    gr.wdone(sp.dma_start(out=g1t[gs][:, :], in_=g1T.ap()[jo * 128:(jo + 1) * 128, t0:t0 + TB]), 16)
                    gr.wdone(sp.dma_start(out=g2t[gs][:, :], in_=g2T.ap()[jo * 128:(jo + 1) * 128, t0:t0 + TB]), 16)
                    gr.wcommit()
                    p1 = pr.wslot(k)
                    for kc in range(SC):
                        last = pe.matmul(p1[:, 0:TB], lhsT=ws[:, kc, jj * 128:(jj + 1) * 128], rhs=ysb[:, kc, :], start=(kc == 0), stop=(kc == SC - 1))
                    pr.wdone(last)
                    p2 = pr.wslot(k)
                    for kc in range(FCn):
                        last = pe.matmul(p2[:, 0:TB], lhsT=wf[:, kc, jj * 128:(jj + 1) * 128], rhs=yfb[:, kc, :], start=(kc == 0), stop=(kc == FCn - 1))
                    pr.wdone(last)
                    gs = gr.rslot(dve)
                    ts_ = tr.wslot(dve)
                    p1 = pr.rslot(k, dve)
                    i1 = dve.tensor_tensor(out=t1[ts_][:, :], in0=p1[:, 0:TB], in1=g1t[gs][:, :], op=ALU.mult)
                    pr.rdone(i1)
                    p2 = pr.rslot(k, dve)
                    i2 = dve.tensor_tensor(out=t2[ts_][:, :], in0=p2[:, 0:TB], in1=g2t[gs][:, :], op=ALU.mult)
                    pr.rdone(i2)
                    gr.rdone(i2)
                    tr.wdone(i2)
                    tr.wcommit()
                    ts_ = tr.rslot(pool)
                    lastp = pool.tensor_tensor(out=mT[:, jo, :], in0=t1[ts_][:, :], in1=t2[ts_][:, :], op=ALU.add)
                    tr.rdone(lastp)
                lw_s.done(last)
                lw_f.done(last)
            sig(last, yfree)
            yneed = yfree.v
            sig(lastp, ms)
            pe.wait_ge(ms.h, ms.v)
            for jg in range(0, KC, 2):
                lw_o.load(k, w_out, 0, jg * 128)
                wo = lw_o.get(k)
                for jj in range(2):
                    jo = jg + jj
                    xs = xr.wslot(sp)
                    xr.wdone(sp.dma_start(out=xt[xs][:, :], in_=xres.ap()[jo * 128:(jo + 1) * 128, t0:t0 + TB]), 16)
                    xr.wcommit()
                    ps = pr.wslot(k)
                    for kc in range(KC):
                        last = pe.matmul(ps[:, 0:TB], lhsT=wo[:, kc, jj * 128:(jj + 1) * 128], rhs=mT[:, kc, :], start=(kc == 0), stop=(kc == KC - 1))
                    pr.wdone(last)
                    ps = pr.rslot(k, dve)
                    xs = xr.rslot(dve)
                    o = st.wslot(dve)
                    ins = dve.scalar_tensor_tensor(out=o[:, :], in0=ps[:, 0:TB], scalar=gcol[:, jo:jo + 1], in1=xt[xs][:, :], op0=ALU.mult, op1=ALU.add)
                    pr.rdone(ins)
                    xr.rdone(ins)
                    st.store(k, ins, xres.ap()[jo * 128:(jo + 1) * 128, t0:t0 + TB])
                lw_o.done(last)
            sig(last, mfree)
            mneed = mfree.v
        st.drain(k)
    k.barrier()


def phase_moe_a(k, tag, xsrc, L, Acol, Bcol, wr_in, li, consts, hx2_tok, affT):
    nc, c = k.nc, k.cfg
    pe, act, dve, pool, sp = k.pe, k.act, k.dve, k.pool, k.sp
    KC, E = c.KC, c.E
    TB = min(512, L)
    NTT = TB // 128
    with ExitStack() as es:
        def sb(name, shape, dt=F32):
            return es.enter_context(nc.sbuf_tensor(f"{tag}_{name}", shape, dt))
        nr = NormRes(k, es, tag + "n", TB)
        wr32 = sb("wr32", [128, KC, E])
        wr = sb("wr", [128, KC, E], BF16)
        ld = k.sem(tag + "ld")
        sig(sp.dma_start(out=wr32[:, :, :], in_=wr_in.ap()[li].rearrange("(kc p) e -> p kc e", p=128)), ld, 16)
        dve.wait_ge(ld.h, ld.v)
        k.fence(dve, dve.tensor_copy(out=wr[:, :, :], in_=wr32[:, :, :]))
        wrs = k.sem(tag + "wrs")
        sig(dve.memset(k.scr[:, 0:1], 0.0), wrs)
        pe.wait_ge(wrs.h, wrs.v)
        z = sb("z", [128, NTT, E])
        ez = sb("ez", [128, NTT, E])
        mx = sb("mx", [128, NTT])
        sm = sb("sm", [128, NTT])
        aff = sb("aff", [128, NTT, E])
        hrow = [sb(f"hrow{i}", [128, c.D], BF16) for i in range(2)]
        hring = Ring(k, tag + "hr", 2)
        pr = PsRing(k, tag + "p", [1, 2, 3, 4])
        s_lp, s_da, s_ad, s_dp, s_pt, s_tf = (k.sem(tag + n_) for n_ in ("lp", "da", "ad", "dp", "pt", "tf"))
        tf_need = 0
        psl, pst = k.ps[5], k.ps[6]
        ev = 0
        for tb in range(L // TB):
            t0 = tb * TB
            hv = emit_norm(k, nr, xsrc, t0, Acol, Bcol, consts["ones32"], consts["eps"], k.ps[0])
            pe.wait_ge(nr.h_full.h, hv)
            if tf_need:
                pe.wait_ge(s_tf.h, tf_need)
            last = None
            for tt in range(NTT):
                for kc in range(KC):
                    last = pe.matmul(psl[:, tt * E:(tt + 1) * E], lhsT=nr.hT[:, kc, tt * 128:(tt + 1) * 128], rhs=wr[:, kc, :],
                                     start=(kc == 0), stop=(kc == KC - 1))
            sig(last, s_lp)
            dve.wait_ge(s_lp.h, s_lp.v)
            lg = psl[:, 0:NTT * E].rearrange("p (t e) -> p t e", e=E)
            k.fence(dve, dve.tensor_reduce(out=mx[:, :], in_=lg, axis=mybir.AxisListType.X, op=ALU.max))
            sig(dve.tensor_tensor(out=z[:, :, :], in0=lg, in1=bc_last(mx[:, :], E), op=ALU.subtract), s_da)
            act.wait_ge(s_da.h, s_da.v)
            sig(act.activation(out=ez[:, :, :], in_=z[:, :, :], func=AF.Exp), s_ad)
            dve.wait_ge(s_ad.h, s_ad.v)
            k.fence(dve, dve.tensor_reduce(out=sm[:, :], in_=ez[:, :, :], axis=mybir.AxisListType.X, op=ALU.add))
            k.fence(dve, dve.reciprocal(out=sm[:, :], in_=sm[:, :]))
            sig(dve.tensor_tensor(out=aff[:, :, :], in0=ez[:, :, :], in1=bc_last(sm[:, :], E), op=ALU.mult), s_dp)
            pe.wait_ge(s_dp.h, s_dp.v)
            for tt in range(NTT):
                last = pe.transpose(out=pst[0:E, tt * 128:(tt + 1) * 128], in_=aff[:, tt, :], identity=consts["ident32"][:, :])
            sig(last, s_pt)
            act.wait_ge(s_pt.h, s_pt.v)
            sig(act.activation(out=affT[:, t0:t0 + TB], in_=pst[0:E, 0:TB], func=AF.Copy), s_tf)
            tf_need = s_tf.v
            for tt in range(NTT):
                hs = hring.wslot(dve, act)
                ins = None
                for kb in range(0, KC, 8):
                    nb = min(8, KC - kb)
                    ps = pr.wslot(k)
                    psb = ps[:, :].bitcast(BF16)
                    for j in range(nb):
                        last = pe.transpose(out=psb[:, j * 128:(j + 1) * 128], in_=nr.hT[:, kb + j, tt * 128:(tt + 1) * 128],
                                            identity=consts["ident16"][:, :])
                    pr.wdone(last)
                    eng = dve if ev % 2 == 0 else act
                    ev += 1
                    ps = pr.rslot(k, eng)
                    psb = ps[:, :].bitcast(BF16)
                    if eng is dve:
                        ins = dve.tensor_copy(out=hrow[hs][:, kb * 128:(kb + nb) * 128], in_=psb[:, 0:nb * 128])
                    else:
                        ins = act.activation(out=hrow[hs][:, kb * 128:(kb + nb) * 128], in_=psb[:, 0:nb * 128], func=AF.Copy)
                    pr.rdone(ins)
                    hring.wdone(ins)
                hring.wcommit()
                hs = hring.rslot(sp)
                dd = sp.dma_start(out=hx2_tok.ap()[t0 + tt * 128:t0 + (tt + 1) * 128, :], in_=hrow[hs][:, :])
                sig(dd, hring.free[hs], 16)
                hring.need[hs] = hring.free[hs].v
            norm_consumed(k, nr, last)
        for s in range(2):
            if hring.need[s]:
                sp.wait_ge(hring.free[s].h, hring.need[s])
    k.barrier()


def phase_moe_b(k, tag, L, affT, consts, idxTi, gatesT):
    nc, c = k.nc, k.cfg
    pe, act, dve = k.pe, k.act, k.dve
    E = c.E
    cap = 2 * L // E
    SLT = min(128, cap)
    NST = cap // SLT
    with ExitStack() as es:
        def sb(name, shape, dt=F32):
            return es.enter_context(nc.sbuf_tensor(f"{tag}_{name}", shape, dt))
        wk = sb("wk", [E, L])
        vals = sb("vals", [E, cap])
        idx = sb("idx", [E, cap], U32)
        idxf = sb("idxf", [E, cap])
        k.fence(dve, dve.tensor_copy(out=wk[:, :], in_=affT[:, :]))
        for r in range(cap // 8):
            k.fence(dve, dve.max(out=vals[:, r * 8:(r + 1) * 8], in_=wk[:, :]))
            k.fence(dve, dve.max_index(out=idx[:, r * 8:(r + 1) * 8], in_max=vals[:, r * 8:(r + 1) * 8], in_values=wk[:, :]))
            k.fence(dve, dve.match_replace(out=wk[:, :], in_to_replace=vals[:, r * 8:(r + 1) * 8], in_values=wk[:, :], imm_value=-1.0))
        s1 = k.sem(tag + "s1")
        sig(dve.tensor_copy(out=idxf[:, :], in_=idx[:, :]), s1)
        pe.wait_ge(s1.h, s1.v)
        s2 = k.sem(tag + "s2")
        s3 = k.sem(tag + "s3")
        ps = k.ps[1]
        for st in range(NST):
            pe.transpose(out=ps[0:SLT, st * E:(st + 1) * E], in_=idxf[:, st * SLT:(st + 1) * SLT], identity=consts["ident32"][0:E, 0:E])
            pe.transpose(out=ps[0:SLT, (NST + st) * E:(NST + st + 1) * E], in_=vals[:, st * SLT:(st + 1) * SLT],
                         identity=consts["ident32"][0:E, 0:E])
        sig(pe.matmul(k.ps[7][0:8, 0:8], lhsT=k.scrb[:, 0:8], rhs=k.scrb[:, 0:8], start=True, stop=True), s2)
        dve.wait_ge(s2.h, s2.v)
        k.fence(dve, dve.tensor_copy(out=idxTi[0:SLT, :, :], in_=ps[0:SLT, 0:NST * E].rearrange("p (s e) -> p s e", e=E)))
        k.fence(dve, dve.tensor_copy(out=gatesT[0:SLT, :, :], in_=ps[0:SLT, NST * E:2 * NST * E].rearrange("p (s e) -> p s e", e=E)))
    k.barrier()


def phase_moe_c(k, tag, L, li, Wd, hx2_tok, idxTi, gatesT, moe_out, consts):
    nc, c = k.nc, k.cfg
    pe, act, dve, pool, sp = k.pe, k.act, k.dve, k.pool, k.sp
    KC, E, FC, D = c.KC, c.E, c.FC, c.D
    cap = 2 * L // E
    SLT = min(128, cap)
    NST = cap // SLT
    w_gate, w_up, w_down = Wd[("w_gate", li)], Wd[("w_up", li)], Wd[("w_down", li)]
    KH = max(1, KC // 2)
    NKH = KC // KH
    GW = min(256, c.F)
    DB = min(512, D)
    with ExitStack() as es:
        def sb(name, shape, dt=BF16):
            return es.enter_context(nc.sbuf_tensor(f"{tag}_{name}", shape, dt))
        zt = sb("zero", [128, D], F32)
        zs = k.sem(tag + "z")
        sig(dve.memset(zt[:, :], 0.0), zs)
        sp.wait_ge(zs.h, zs.v)
        zd = k.sem(tag + "zd")
        CW = D // len(moe_out)
        for r0 in range(0, L, 128):
            for h_, mo_t in enumerate(moe_out):
                sig(sp.dma_start(out=mo_t.ap()[r0:r0 + 128, :], in_=zt[:, h_ * CW:(h_ + 1) * CW]), zd, 16)
        pool.wait_ge(zd.h, zd.v)
        xg = [sb(f"xg{i}", [128, D]) for i in range(2)]
        xgr = Ring(k, tag + "xg", 2)
        xgT = sb("xgT", [128, KC, cap])
        xgT_full = k.sem(tag + "xtf")
        xgT_free = k.sem(tag + "xte")
        xgT_need = 0
        hidT = sb("hidT", [128, FC, cap])
        hid_full = k.sem(tag + "hf")
        hid_free = k.sem(tag + "he")
        hid_need = 0
        lw_g = LinW(k, es, tag + "wg", KH, GW, 2)
        lw_u = LinW(k, es, tag + "wu", KH, GW, 2)
        lw_d = LinW(k, es, tag + "wd", FC, DB, 2)
        sgt = [sb(f"sg{i}", [128, cap], F32) for i in range(2)]
        sgr = Ring(k, tag + "sg", 2)
        ye = sb("ye", [128, NST, D], F32)
        ye_full = k.sem(tag + "yf")
        sc_done = k.sem(tag + "scd")
        pr = PsRing(k, tag + "p", [1, 2, 3, 4, 5, 6])
        ev = 0
        for e in range(E):
            if xgT_need:
                dve.wait_ge(xgT_free.h, xgT_need)
                act.wait_ge(xgT_free.h, xgT_need)
            lastc = []
            for st in range(NST):
                s = xgr.wslot(pool)
                g = pool.indirect_dma_start(out=xg[s][0:SLT, :], out_offset=None, in_=hx2_tok.ap()[:, :],
                                            in_offset=bass.IndirectOffsetOnAxis(ap=idxTi[0:SLT, st, e:e + 1], axis=0))
                xgr.wdone(g, 16)
                xgr.wcommit()
                s = xgr.rslot(pe)
                last = None
                for kb in range(0, KC, 8):
                    nb = min(8, KC - kb)
                    ps = pr.wslot(k)
                    psb = ps[:, :].bitcast(BF16)
                    for j in range(nb):
                        last = pe.transpose(out=psb[:, j * SLT:(j + 1) * SLT], in_=xg[s][0:SLT, (kb + j) * 128:(kb + j + 1) * 128],
                                            identity=consts["ident16"][0:SLT, 0:SLT])
                    pr.wdone(last)
                    eng = dve if ev % 2 == 0 else act
                    ev += 1
                    ps = pr.rslot(k, eng)
                    psv = ps[:, :].bitcast(BF16)[:, 0:nb * SLT].rearrange("p (j s) -> p j s", s=SLT)
                    if eng is dve:
                        ins = dve.tensor_copy(out=xgT[:, kb:kb + nb, st * SLT:(st + 1) * SLT], in_=psv)
                    else:
                        ins = act.activation(out=xgT[:, kb:kb + nb, st * SLT:(st + 1) * SLT], in_=psv, func=AF.Copy)
                    pr.rdone(ins)
                    lastc = [ins] + lastc[:1]
                xgr.rdone(last)
            for ins in lastc:
                sig(ins, xgT_full)
            pe.wait_ge(xgT_full.h, xgT_full.v)
            if hid_need:
                dve.wait_ge(hid_free.h, hid_need)
            lastd = None
            last = None
            nf = GW // 128
            for fg in range(0, FC, nf):
                slots = []
                for f in range(nf):
                    sg_ = pr.ring.wslot(pe)
                    su_ = pr.ring.wslot(pe)
                    slots.append((sg_, su_))
                for kh in range(NKH):
                    lw_g.load(k, w_gate, e * D + kh * KH * 128, fg * 128)
                    lw_u.load(k, w_up, e * D + kh * KH * 128, fg * 128)
                    wg, wu = lw_g.get(k), lw_u.get(k)
                    for f in range(nf):
                        pg_, pu_ = k.ps[pr.banks[slots[f][0]]], k.ps[pr.banks[slots[f][1]]]
                        for kk in range(KH):
                            kc = kh * KH + kk
                            pe.matmul(pg_[:, 0:cap], lhsT=wg[:, kk, f * 128:(f + 1) * 128], rhs=xgT[:, kc, :],
                                      start=(kc == 0), stop=(kc == KC - 1))
                        for kk in range(KH):
                            kc = kh * KH + kk
                            last = pe.matmul(pu_[:, 0:cap], lhsT=wu[:, kk, f * 128:(f + 1) * 128], rhs=xgT[:, kc, :],
                                             start=(kc == 0), stop=(kc == KC - 1))
                    lw_g.done(last)
                    lw_u.done(last)
                for f in range(nf):
                    for sl in slots[f]:
                        pr.ring.cur_w = sl
                        pr.ring.wdone(last)
                        pr.ring.wcommit()
                for f in range(nf):
                    fo = fg + f
                    pgs = pr.rslot(k, act)
                    s = sgr.wslot(act)
                    a = act.activation(out=sgt[s][:, :], in_=pgs[:, 0:cap], func=AF.Silu)
                    pr.rdone(a)
                    sgr.wdone(a)
                    sgr.wcommit()
                    pus = pr.rslot(k, dve)
                    s = sgr.rslot(dve)
                    lastd = dve.tensor_tensor(out=hidT[:, fo, :], in0=pus[:, 0:cap], in1=sgt[s][:, :], op=ALU.mult)
                    pr.rdone(lastd)
                    sgr.rdone(lastd)
            sig(last, xgT_free)
            xgT_need = xgT_free.v
            sig(lastd, hid_full)
            pe.wait_ge(hid_full.h, hid_full.v)
            if e > 0:
                dve.wait_ge(sc_done.h, sc_done.v)
                act.wait_ge(sc_done.h, sc_done.v)
            lasts = []
            for db in range(D // DB):
                lw_d.load(k, w_down, e * c.F, db * DB)
                wd = lw_d.get(k)
                for st in range(NST):
                    ps = pr.wslot(k)
                    for f in range(FC):
                        last = pe.matmul(ps[0:SLT, 0:DB], lhsT=hidT[:, f, st * SLT:(st + 1) * SLT], rhs=wd[:, f, :],
                                         start=(f == 0), stop=(f == FC - 1))
                    pr.wdone(last)
                    eng = dve if ev % 2 == 0 else act
                    ev += 1
                    ps = pr.rslot(k, eng)
                    if eng is dve:
                        ins = dve.tensor_scalar(out=ye[0:SLT, st, db * DB:(db + 1) * DB], in0=ps[0:SLT, 0:DB],
                                                scalar1=gatesT[0:SLT, st, e:e + 1], scalar2=None, op0=ALU.mult)
                    else:
                        ins = act.activation(out=ye[0:SLT, st, db * DB:(db + 1) * DB], in_=ps[0:SLT, 0:DB], func=AF.Copy,
                                             scale=gatesT[0:SLT, st, e:e + 1])
                    pr.rdone(ins)
                    lasts = [ins] + lasts[:1]
                lw_d.done(last)
            sig(last, hid_free)
            hid_need = hid_free.v
            for ins in lasts:
                sig(ins, ye_full)
            pool.wait_ge(ye_full.h, ye_full.v)
            for st in range(NST):
                if sc_done.v:
                    pool.wait_ge(sc_done.h, sc_done.v)
                for h_, mo_t in enumerate(moe_out):
                    if h_ > 0:
                        pool.wait_ge(sc_done.h, sc_done.v)
                    d_ = pool.indirect_dma_start(out=mo_t.ap()[:, :], out_offset=bass.IndirectOffsetOnAxis(ap=idxTi[0:SLT, st, e:e + 1], axis=0),
                                                 in_=ye[0:SLT, st, h_ * CW:(h_ + 1) * CW], in_offset=None, compute_op=ALU.add)
                    sig(d_, sc_done, 16)
        pool.wait_ge(sc_done.h, sc_done.v)
    k.barrier()


def phase_moe_d(k, tag, L, moe_out, xres, gcol, consts):
    nc, c = k.nc, k.cfg
    pe, act, dve, sp = k.pe, k.act, k.dve, k.sp
    KC, D = c.KC, c.D
    TB = min(512, L)
    NTT = TB // 128
    with ExitStack() as es:
        def sb(name, shape, dt=F32):
            return es.enter_context(nc.sbuf_tensor(f"{tag}_{name}", shape, dt))
        mo = [sb(f"mo{i}", [128, NTT, D]) for i in range(1)]
        mo_full = k.sem(tag + "mf")
        mo_free = k.sem(tag + "me")
        mo_need = 0
        xt = [sb(f"x{i}", [128, TB]) for i in range(2)]
        xr = Ring(k, tag + "x", 2)
        pr = PsRing(k, tag + "p", [1, 2, 3, 4])
        st = OutStage(k, es, tag + "o", [128, TB], F32, 3)
        for tb in range(L // TB):
            t0 = tb * TB
            if mo_need:
                sp.wait_ge(mo_free.h, mo_need)
            CW = D // len(moe_out)
            for tt in range(NTT):
                for h_, mo_t in enumerate(moe_out):
                    sig(sp.dma_start(out=mo[0][:, tt, h_ * CW:(h_ + 1) * CW], in_=mo_t.ap()[t0 + tt * 128:t0 + (tt + 1) * 128, :]), mo_full, 16)
            pe.wait_ge(mo_full.h, mo_full.v)
            last = None
            for kc in range(KC):
                xs = xr.wslot(sp)
                xr.wdone(sp.dma_start(out=xt[xs][:, :], in_=xres.ap()[kc * 128:(kc + 1) * 128, t0:t0 + TB]), 16)
                xr.wcommit()
                ps = pr.wslot(k)
                for tt in range(NTT):
                    last = pe.transpose(out=ps[:, tt * 128:(tt + 1) * 128], in_=mo[0][:, tt, kc * 128:(kc + 1) * 128],
                                        identity=consts["ident32"][:, :])
                pr.wdone(last)
                ps = pr.rslot(k, dve)
                xs = xr.rslot(dve)
                o = st.wslot(dve)
                ins = dve.scalar_tensor_tensor(out=o[:, :], in0=ps[:, 0:TB], scalar=gcol[:, kc:kc + 1], in1=xt[xs][:, :],
                                               op0=ALU.mult, op1=ALU.add)
                pr.rdone(ins)
                xr.rdone(ins)
                st.store(k, ins, xres.ap()[kc * 128:(kc + 1) * 128, t0:t0 + TB])
            sig(last, mo_free)
            mo_need = mo_free.v
        st.drain(k)
    k.barrier()


def phase_final(k, tag, xres, L, gfin, consts, outT):
    nc, c = k.nc, k.cfg
    TB = min(512, L)
    with ExitStack() as es:
        nr = NormRes(k, es, tag + "n", TB, out_dt=F32)
        zero = es.enter_context(nc.sbuf_tensor(tag + "_zero", [128, c.KC], F32))
        k.fence(k.dve, k.dve.memset(zero[:, :], 0.0))
        zs = k.sem(tag + "z")
        sig(k.dve.memset(k.scr[:, 0:1], 0.0), zs)
        k.act.wait_ge(zs.h, zs.v)
        ds = k.sem(tag + "d")
        ov = outT.ap().rearrange("(kc p) l -> p kc l", p=128)
        for tb in range(L // TB):
            t0 = tb * TB
            hv = emit_norm(k, nr, xres, t0, gfin, zero, consts["ones32"], consts["eps"], k.ps[0])
            k.sp.wait_ge(nr.h_full.h, hv)
            d_ = k.sp.dma_start(out=ov[:, :, t0:t0 + TB], in_=nr.hT[:, :, :])
            sig(d_, nr.h_free, 16)
            nr.h_need = nr.h_free.v
            sig(d_, ds, 16) if False else None
        k.sp.wait_ge(nr.h_free.h, nr.h_need)
    k.barrier()


def phase_posembed(k, tag, xin, xres, L, rowcol, invf, consts):
    nc, c = k.nc, k.cfg
    act, dve, pool, sp = k.act, k.dve, k.pool, k.sp
    KC = c.KC
    TB = min(512, L)
    qc = (c.D // 4) // 128
    with ExitStack() as es:
        def sb(name, shape, dt=F32):
            return es.enter_context(nc.sbuf_tensor(f"{tag}_{name}", shape, dt))
        xt = [sb(f"x{i}", [128, TB]) for i in range(3)]
        xr = Ring(k, tag + "x", 3)
        ang = [sb(f"a{i}", [128, TB]) for i in range(2)]
        aab = [sb(f"b{i}", [128, TB]) for i in range(2)]
        ar = Ring(k, tag + "a", 2)
        tf = sb("tf", [128, TB])
        ti = sb("ti", [128, TB], I32)
        pt = [sb(f"p{i}", [128, TB]) for i in range(2)]
        prg = Ring(k, tag + "p", 2)
        st = OutStage(k, es, tag + "o", [128, TB], F32, 3)
        for tb in range(L // TB):
            t0 = tb * TB
            for kc in range(KC):
                quarter = kc // qc
                xs = xr.wslot(sp)
                xr.wdone(sp.dma_start(out=xt[xs][:, :], in_=xin.ap()[kc * 128:(kc + 1) * 128, t0:t0 + TB]), 16)
                xr.wcommit()
                a_s = ar.wslot(dve)
                dve.tensor_scalar(out=ang[a_s][:, :], in0=rowcol[:, quarter // 2, t0:t0 + TB], scalar1=invf[:, kc:kc + 1], scalar2=None,
                                  op0=ALU.mult)
                i1 = emit_range_reduce(dve, ang[a_s][:, :], tf[:, :], ti[:, :], aab[a_s][:, :])
                ar.wdone(i1)
                ar.wcommit()
                a_s = ar.rslot(act)
                ps_ = prg.wslot(act)
                if quarter % 2 == 0:
                    a = act.activation(out=pt[ps_][:, :], in_=ang[a_s][:, :], func=AF.Sin)
                else:
                    a = act.activation(out=pt[ps_][:, :], in_=aab[a_s][:, :], func=AF.Sin, scale=-1.0, bias=consts["hpi"][:, 0:1])
                ar.rdone(a)
                prg.wdone(a)
                prg.wcommit()
                ps_ = prg.rslot(dve)
                xs = xr.rslot(dve)
                o = st.wslot(dve)
                ins = dve.tensor_tensor(out=o[:, :], in0=xt[xs][:, :], in1=pt[ps_][:, :], op=ALU.add)
                prg.rdone(ins)
                xr.rdone(ins)
                st.store(k, ins, xres.ap()[kc * 128:(kc + 1) * 128, t0:t0 + TB])
        st.drain(k)
    k.barrier()


_PROGRAM_CACHE = {}


def kernel(**inputs):
    cfg = Cfg()
    if "nc" not in _PROGRAM_CACHE:
        _PROGRAM_CACHE["nc"] = build_program(cfg)[0]
    nc = _PROGRAM_CACHE["nc"]
    maps = prepare_in_maps(cfg, inputs)
    res = run_bass_kernel_spmd(nc, maps, core_ids=list(range(NCORES)))
    nb = np.asarray(inputs["x"]).shape[0]
    out = np.stack([np.ascontiguousarray(np.asarray(res.results[b]["outT"], dtype=np.float32).T) for b in range(nb)], 0)
    return out
```
